# Optimizing a Trainium2 kernel written in Bass

```python
import jax, jax.numpy as jnp
from jax import lax
import numpy as np

D_MODEL = 2048
BATCH = 2
SEQ = 4096
DEPTH = 2
DEC_BATCH = 16
DEC_SEQ = 64
PAST_LEN = 2048

CHUNK = 64
N_HEADS = 16
N_KV_HEADS = 4
HEAD_DIM = 64
GROUP = N_HEADS // N_KV_HEADS
WINDOW = 128
BAND_CHUNKS = WINDOW // CHUNK + 1
ROT_DIM = HEAD_DIM // 4
ROPE_THETA = 500000.0
ATTN_SCALE = HEAD_DIM ** -0.5
POOL_WINDOWS = (2, 4, 8, 16)
N_POOL_GROUPS = 4
POOL_W = 1024
POOL_GROUP_W = POOL_W // N_POOL_GROUPS
POOL_HIST = max(POOL_WINDOWS) - 1
ATTN_W = N_HEADS * HEAD_DIM
KV_W = N_KV_HEADS * HEAD_DIM
IN_W = ATTN_W + 2 * KV_W + POOL_W + 2 * D_MODEL
SPLIT_AT = (ATTN_W, ATTN_W + KV_W, ATTN_W + 2 * KV_W, ATTN_W + 2 * KV_W + POOL_W,
            ATTN_W + 2 * KV_W + POOL_W + D_MODEL)
D_FF = 5632
N_EXPERTS = 8
TOP_K = 2
D_FF_EXPERT = 5632
PLE_DIM = 256
N_DENSE = (DEPTH + 1) // 2
N_MOE = DEPTH // 2
RMS_EPS = 1e-6
NEG_INF = -1e30

kernel_name = "hybrid_swa_pool_stream_encoder_step"


def rms_norm(x, g):
    xf = x.astype(jnp.float32)
    y = xf * lax.rsqrt(jnp.mean(xf * xf, axis=-1, keepdims=True) + RMS_EPS)
    return (y * g.astype(jnp.float32)).astype(x.dtype)


def apply_partial_rope(x, pos):
    inv = ROPE_THETA ** (-jnp.arange(0, ROT_DIM, 2, dtype=jnp.float32) / ROT_DIM)
    ang = pos.astype(jnp.float32)[:, None] * inv[None, :]
    cos = jnp.concatenate([jnp.cos(ang), jnp.cos(ang)], -1)[None, :, None, :]
    sin = jnp.concatenate([jnp.sin(ang), jnp.sin(ang)], -1)[None, :, None, :]
    xr = x[..., :ROT_DIM].astype(jnp.float32)
    x1, x2 = xr[..., :ROT_DIM // 2], xr[..., ROT_DIM // 2:]
    rot = xr * cos + jnp.concatenate([-x2, x1], -1) * sin
    return jnp.concatenate([rot.astype(x.dtype), x[..., ROT_DIM:]], -1)


def sink_attention(q, k, v, valid, sinks_l):
    s = jnp.einsum('bncxgd,bnlxd->bnxgcl', q, k).astype(jnp.float32) * ATTN_SCALE
    s = jnp.where(valid[None, :, None, None, None, :], s, NEG_INF)
    sink = jnp.broadcast_to(sinks_l.astype(jnp.float32).reshape(1, 1, N_KV_HEADS, GROUP, 1, 1),
                            s.shape[:-1] + (1,))
    p = jax.nn.softmax(jnp.concatenate([s, sink], axis=-1), axis=-1)[..., :-1].astype(v.dtype)
    return jnp.einsum('bnxgcl,bnlxd->bncxgd', p, v)


def banded_window_attention(q, k, v, sinks_l):
    b, s = q.shape[:2]
    nc = s // CHUNK
    qc = q.reshape(b, nc, CHUNK, N_KV_HEADS, GROUP, HEAD_DIM)
    pad = ((0, 0), ((BAND_CHUNKS - 1) * CHUNK, 0), (0, 0), (0, 0))
    kp = jnp.pad(k, pad).reshape(b, nc + BAND_CHUNKS - 1, CHUNK, N_KV_HEADS, HEAD_DIM)
    vp = jnp.pad(v, pad).reshape(b, nc + BAND_CHUNKS - 1, CHUNK, N_KV_HEADS, HEAD_DIM)
    kb = jnp.concatenate([kp[:, j:j + nc] for j in range(BAND_CHUNKS)], axis=2)
    vb = jnp.concatenate([vp[:, j:j + nc] for j in range(BAND_CHUNKS)], axis=2)
    key_chunk = (jnp.arange(nc)[:, None] + jnp.arange(BAND_CHUNKS * CHUNK)[None, :] // CHUNK
                 - (BAND_CHUNKS - 1))
    o = sink_attention(qc, kb, vb, key_chunk >= 0, sinks_l)
    return o.reshape(b, s, ATTN_W)


def pool_mixer(u, hist, pos0, pool_w_l, pool_scale_l):
    b, L, _ = u.shape
    ext = jnp.concatenate([hist, u], axis=1).astype(jnp.float32)
    cs = jnp.concatenate([jnp.zeros((b, 1, POOL_W), jnp.float32), jnp.cumsum(ext, axis=1)], axis=1)
    pos = pos0 + jnp.arange(L, dtype=jnp.int32)
    means = []
    for g, w in enumerate(POOL_WINDOWS):
        sl = slice(g * POOL_GROUP_W, (g + 1) * POOL_GROUP_W)
        tot = cs[:, POOL_HIST + 1:, sl] - cs[:, POOL_HIST + 1 - w:POOL_HIST + 1 - w + L, sl]
        cnt = jnp.minimum(w, pos + 1).astype(jnp.float32)[None, :, None]
        means.append(tot / cnt)
    d = (jnp.concatenate(means, axis=-1) - u.astype(jnp.float32)).astype(u.dtype)
    d = d.reshape(b, L, N_POOL_GROUPS, POOL_GROUP_W)
    y = jnp.einsum('blgc,gcd->blgd', d, pool_w_l).reshape(b, L, POOL_W)
    return y * pool_scale_l


def token_mixer(h, pos0, k_hist, v_hist, pool_hist, w_in_l, q_gain, k_gain, sinks_l,
                pool_w_l, pool_scale_l, w_attn_br_l, w_pool_br_l, w_out_l):
    b, L, _ = h.shape
    pos = pos0 + jnp.arange(L, dtype=jnp.int32)
    q, k, v, u, ga, gb = jnp.split(h @ w_in_l, SPLIT_AT, axis=-1)
    q = apply_partial_rope(rms_norm(q.reshape(b, L, N_HEADS, HEAD_DIM), q_gain), pos)
    k = apply_partial_rope(rms_norm(k.reshape(b, L, N_KV_HEADS, HEAD_DIM), k_gain), pos)
    v = v.reshape(b, L, N_KV_HEADS, HEAD_DIM)
    if k_hist is None:
        o = banded_window_attention(q, k, v, sinks_l)
        new_k, new_v = k[:, -WINDOW:], v[:, -WINDOW:]
        hist = jnp.zeros((b, POOL_HIST, POOL_W), u.dtype)
    else:
        k_all = jnp.concatenate([k_hist, k], axis=1)
        v_all = jnp.concatenate([v_hist, v], axis=1)
        qc = q.reshape(b, 1, L, N_KV_HEADS, GROUP, HEAD_DIM)
        valid = jnp.ones((1, k_all.shape[1]), dtype=bool)
        o = sink_attention(qc, k_all[:, None], v_all[:, None], valid, sinks_l).reshape(b, L, ATTN_W)
        new_k, new_v = k_all[:, -WINDOW:], v_all[:, -WINDOW:]
        hist = pool_hist
    pooled = pool_mixer(u, hist, pos0, pool_w_l, pool_scale_l)
    new_pool = jnp.concatenate([hist, u], axis=1)[:, -POOL_HIST:]
    a = o @ w_attn_br_l
    c = pooled @ w_pool_br_l
    out = (jax.nn.sigmoid(ga) * a + jax.nn.sigmoid(gb) * c) @ w_out_l
    return out, new_k, new_v, new_pool


def swiglu(h, w_gate_up, w_down):
    g, u = jnp.split(h @ w_gate_up, 2, axis=-1)
    return (jax.nn.silu(g) * u) @ w_down


def moe_ffn(h, router, w_gate_up, w_down):
    logits = (h @ router).astype(jnp.float32)
    top_v, top_i = lax.top_k(logits, TOP_K)
    wts = jax.nn.softmax(top_v, axis=-1)
    combine = jnp.sum(jax.nn.one_hot(top_i, N_EXPERTS, dtype=jnp.float32) * wts[..., None], axis=-2)
    combine = combine.astype(h.dtype)
    out = jnp.zeros_like(h)
    for e in range(N_EXPERTS):
        out = out + combine[..., e:e + 1] * swiglu(h, w_gate_up[e], w_down[e])
    return out


def setup_inputs(seed: int = 0) -> dict:
    key = jax.random.key(seed)
    ks = jax.random.split(key, 32)
    n = lambda i, shape: jax.random.normal(ks[i], shape, jnp.float32)
    return {
        "x_prompt": n(0, (BATCH, SEQ, D_MODEL)),
        "x_sample": n(1, (DEC_BATCH, DEC_SEQ, D_MODEL)),
        "cache_k": n(2, (DEPTH, DEC_BATCH, WINDOW, N_KV_HEADS, HEAD_DIM)),
        "cache_v": n(3, (DEPTH, DEC_BATCH, WINDOW, N_KV_HEADS, HEAD_DIM)),
        "state_pool": n(4, (DEPTH, DEC_BATCH, POOL_HIST, POOL_W)),
        "p_prompt": n(5, (DEPTH, BATCH, SEQ, PLE_DIM)),
        "p_sample": n(6, (DEPTH, DEC_BATCH, DEC_SEQ, PLE_DIM)),
        "norm_mix": 1.0 + 0.1 * n(7, (DEPTH, D_MODEL)),
        "w_in": n(8, (DEPTH, D_MODEL, IN_W)) * D_MODEL ** -0.5,
        "q_norm": 1.0 + 0.1 * n(9, (DEPTH, HEAD_DIM)),
        "k_norm": 1.0 + 0.1 * n(10, (DEPTH, HEAD_DIM)),
        "sinks": 0.5 * n(11, (DEPTH, N_HEADS)),
        "pool_w": n(12, (DEPTH, N_POOL_GROUPS, POOL_GROUP_W, POOL_GROUP_W)) * POOL_GROUP_W ** -0.5,
        "pool_scale": 1.0 + 0.1 * n(13, (DEPTH, POOL_W)),
        "w_attn_br": n(14, (DEPTH, ATTN_W, D_MODEL)) * ATTN_W ** -0.5,
        "w_pool_br": n(15, (DEPTH, POOL_W, D_MODEL)) * POOL_W ** -0.5,
        "w_out": n(16, (DEPTH, D_MODEL, D_MODEL)) * D_MODEL ** -0.5,
        "norm_ffn": 1.0 + 0.1 * n(17, (DEPTH, D_MODEL)),
        "ffn_w_gate_up": n(18, (N_DENSE, D_MODEL, 2 * D_FF)) * D_MODEL ** -0.5,
        "ffn_w_down": n(19, (N_DENSE, D_FF, D_MODEL)) * D_FF ** -0.5,
        "moe_router": n(20, (N_MOE, D_MODEL, N_EXPERTS)) * D_MODEL ** -0.5,
        "moe_w_gate_up": n(21, (N_MOE, N_EXPERTS, D_MODEL, 2 * D_FF_EXPERT)) * D_MODEL ** -0.5,
        "moe_w_down": n(22, (N_MOE, N_EXPERTS, D_FF_EXPERT, D_MODEL)) * D_FF_EXPERT ** -0.5,
        "norm_ple": 1.0 + 0.1 * n(23, (DEPTH, D_MODEL)),
        "w_ple": n(24, (DEPTH, PLE_DIM, D_MODEL)) * PLE_DIM ** -0.5,
        "w_ple_gate": n(25, (DEPTH, D_MODEL, D_MODEL)) * D_MODEL ** -0.5,
    }


def reference(x_prompt, x_sample, cache_k, cache_v, state_pool, p_prompt, p_sample,
              norm_mix, w_in, q_norm, k_norm, sinks, pool_w, pool_scale, w_attn_br, w_pool_br,
              w_out, norm_ffn, ffn_w_gate_up, ffn_w_down, moe_router, moe_w_gate_up, moe_w_down,
              norm_ple, w_ple, w_ple_gate):
    def run_layer(l, x, p_l, pos0, k_hist, v_hist, pool_hist):
        h = rms_norm(x, norm_mix[l])
        mix, nk, nv, npool = token_mixer(h, pos0, k_hist, v_hist, pool_hist, w_in[l], q_norm[l],
                                         k_norm[l], sinks[l], pool_w[l], pool_scale[l],
                                         w_attn_br[l], w_pool_br[l], w_out[l])
        x = x + mix
        h2 = rms_norm(x, norm_ffn[l])
        if l % 2 == 0:
            x = x + swiglu(h2, ffn_w_gate_up[l // 2], ffn_w_down[l // 2])
        else:
            x = x + moe_ffn(h2, moe_router[l // 2], moe_w_gate_up[l // 2], moe_w_down[l // 2])
        gate = jax.nn.sigmoid(rms_norm(x, norm_ple[l]) @ w_ple_gate[l])
        x = x + (p_l @ w_ple[l]) * gate
        return x, nk, nv, npool

    xp, xs = x_prompt, x_sample
    kp_l, vp_l, pp_l, ks_l, vs_l, ps_l = [], [], [], [], [], []
    for l in range(DEPTH):
        xp, nk, nv, npool = run_layer(l, xp, p_prompt[l], 0, None, None, None)
        kp_l.append(nk); vp_l.append(nv); pp_l.append(npool)
        xs, nk, nv, npool = run_layer(l, xs, p_sample[l], PAST_LEN, cache_k[l], cache_v[l],
                                      state_pool[l])
        ks_l.append(nk); vs_l.append(nv); ps_l.append(npool)
    new_k_prompt = jnp.stack(kp_l)
    new_v_prompt = jnp.stack(vp_l)
    new_pool_prompt = jnp.stack(pp_l)
    new_k_sample = jnp.stack(ks_l)
    new_v_sample = jnp.stack(vs_l)
    new_pool_sample = jnp.stack(ps_l)
    return (xp, xs, new_k_prompt, new_v_prompt, new_pool_prompt, new_k_sample, new_v_sample, new_pool_sample)
```

```python
import math
from contextlib import ExitStack

import numpy as np
import ml_dtypes
import concourse.bass as bass
import concourse.mybir as mybir
from concourse.bass_utils import run_bass_kernel_spmd

F32 = mybir.dt.float32
BF16 = mybir.dt.bfloat16
ALU = mybir.AluOpType
AF = mybir.ActivationFunctionType
AX = mybir.AxisListType

NCORES = 8
D = 2048
NCH = 16
DEPTH = 2
TK = 1408
XW = 1280
C_MAIN = 256
C_S = (1280, 1344)
FR = ((128, 1408), (256, 1408))
KVR = ((0, 1408), (128, 1408))
IN_W = 6656
C_Q, C_K, C_V, C_U, C_GA, C_GB = 0, 1024, 1280, 1536, 2560, 4608
DFF = 5632
NHID = 44
NEXP = 8
EPS = 1e-6
ROPE_THETA = 500000.0
PAST = 2048
NEG = -30000.0


def blocks(c0, c1, maxw=512):
    n = -(-(c1 - c0) // maxw)
    w = -(-((c1 - c0) // 128) // n) * 128
    out = []
    c = c0
    while c < c1:
        out.append((c, min(c + w, c1)))
        c += w
    return out


class Tok:
    __slots__ = ("sem", "val")

    def __init__(self, sem, val):
        self.sem = sem
        self.val = val


class Cell:
    __slots__ = ("w", "r")

    def __init__(self):
        self.w = None
        self.r = {}


class Eng:
    def __init__(self, name, sem):
        self.name = name
        self.sem = sem
        self.count = 0
        self.q = []
        self.waited = {}


class DSem:
    def __init__(self, sem):
        self.sem = sem
        self.issued = 0


class Prog:
    def __init__(self, nc, es, debug=None):
        self.nc = nc
        self.es = es
        self.cells = {}
        self.debug = debug or {}
        self.dbg_outs = {}
        mk = lambda n: es.enter_context(nc.semaphore(n))
        self.PE = Eng("pe", mk("s_pe"))
        self.ACT = Eng("act", mk("s_act"))
        self.DVE = Eng("dve", mk("s_dve"))
        self.POOL = Eng("pool", mk("s_pool"))
        self.SP = Eng("sp", mk("s_sp"))
        self.engs = [self.PE, self.ACT, self.DVE, self.POOL, self.SP]
        self.dsems = []
        self.bank_i = 0
        self.tmp_i = 0
        self.rr = 0

    def dsem(self, name):
        d = DSem(self.es.enter_context(self.nc.semaphore(name)))
        self.dsems.append(d)
        return d

    def cell(self, key):
        c = self.cells.get(key)
        if c is None:
            c = Cell()
            self.cells[key] = c
        return c

    def need(self, E, tok):
        if tok is None:
            return
        if E is self.PE and tok.sem is self.PE.sem:
            return
        k = id(tok.sem)
        if E.waited.get(k, 0) >= tok.val:
            return
        E.waited[k] = tok.val
        E.q.append(("w", tok.sem, tok.val))

    def _deps(self, E, reads, writes):
        for key in reads:
            self.need(E, self.cell(key).w)
        for key in writes:
            c = self.cell(key)
            self.need(E, c.w)
            for s, v in c.r.values():
                self.need(E, Tok(s, v))

    def _commit(self, tok, reads, writes):
        k = id(tok.sem)
        for key in reads:
            c = self.cell(key)
            old = c.r.get(k)
            if old is None or old[1] < tok.val:
                c.r[k] = (tok.sem, tok.val)
        for key in writes:
            c = self.cell(key)
            c.w = tok
            c.r = {}

    def op(self, E, fn, reads=(), writes=()):
        self._deps(E, reads, writes)
        E.count += 1
        tok = Tok(E.sem, E.count)
        E.q.append(("i", fn, E.sem, 1))
        self._commit(tok, reads, writes)
        return tok

    def dma(self, E, out, in_, ds, reads=(), writes=(), commit_reads=True):
        self._deps(E, reads, writes)
        ds.issued += 16
        tok = Tok(ds.sem, ds.issued)
        E.q.append(("i", lambda e: e.dma_start(out=out, in_=in_), ds.sem, 16))
        self._commit(tok, reads if commit_reads else (), writes)
        return tok

    def job(self, writes):
        self._deps(self.PE, (), writes)
        self.jwrites = list(writes)

    def mm(self, out, lhsT, rhs, start, stop, reads=(), last=False, inc=False):
        PE = self.PE
        for key in reads:
            self.need(PE, self.cell(key).w)
        tok = Tok(PE.sem, PE.count + 1)
        self._commit(tok, reads, ())
        if last or inc:
            PE.count += 1
            PE.q.append(("i", lambda e: e.matmul(out, lhsT=lhsT, rhs=rhs, start=start, stop=stop), PE.sem, 1))
            if last:
                self._commit(tok, (), self.jwrites)
        else:
            PE.q.append(("i", lambda e: e.matmul(out, lhsT=lhsT, rhs=rhs, start=start, stop=stop), None, 0))

    def group_commit(self, ds, keys):
        tok = Tok(ds.sem, ds.issued)
        for key in keys:
            self.cell(key).w = tok

    def barrier(self, engs=None):
        engs = engs or [self.PE, self.ACT, self.DVE, self.SP]
        for E in engs:
            for F in [self.PE, self.ACT, self.DVE]:
                if F.count > 0:
                    self.need(E, Tok(F.sem, F.count))
            for d in self.dsems:
                if d.issued > 0 and not getattr(d, "ring", False):
                    self.need(E, Tok(d.sem, d.issued))

    def act(self, out, in_, func, reads, writes, bias=None, scale=None):
        kw = {}
        if bias is not None:
            kw["bias"] = bias
        if scale is not None:
            kw["scale"] = scale
        return self.op(self.ACT, lambda e: e.activation(out=out, in_=in_, func=func, **kw), reads, writes)

    def tt(self, out, in0, in1, op, reads, writes, E=None):
        return self.op(E or self.DVE, lambda e: e.tensor_tensor(out=out, in0=in0, in1=in1, op=op), reads, writes)

    def ts(self, out, in0, s1, op0, reads, writes, s2=None, op1=None, E=None):
        if op1 is None:
            return self.op(E or self.DVE, lambda e: e.tensor_scalar(out=out, in0=in0, scalar1=s1, scalar2=None, op0=op0),
                           reads, writes)
        return self.op(E or self.DVE,
                       lambda e: e.tensor_scalar(out=out, in0=in0, scalar1=s1, scalar2=s2, op0=op0, op1=op1), reads, writes)

    def stt(self, out, in0, scalar, in1, op0, op1, reads, writes, E=None):
        return self.op(E or self.DVE,
                       lambda e: e.scalar_tensor_tensor(out=out, in0=in0, scalar=scalar, in1=in1, op0=op0, op1=op1),
                       reads, writes)

    def copy(self, E, out, in_, reads, writes):
        if E is self.ACT:
            return self.op(E, lambda e: e.copy(out=out, in_=in_), reads, writes)
        return self.op(E, lambda e: e.tensor_copy(out=out, in_=in_), reads, writes)

    def memset(self, E, ap, val, writes):
        return self.op(E, lambda e: e.memset(ap, val), (), writes)

    def alt(self):
        self.rr ^= 1
        return self.ACT if self.rr else self.DVE


def cks(name, chunk, c0, c1):
    return [(name, chunk, cb) for cb in range(c0 // 128, -(-c1 // 128))]


def build_program(debug=None, stop_after=None, nlayers=DEPTH, tiny_moe=False):
    debug = debug or {}
    nc = bass.Bass("TRN2", target_bir_lowering=False)
    es = ExitStack()

    def din(name, shape, dt=F32):
        return nc.dram_tensor(name, list(shape), dt, kind="ExternalInput").ap()

    def dout(name, shape, dt=F32):
        return nc.dram_tensor(name, list(shape), dt, kind="ExternalOutput").ap()

    xin = din("xin", [TK, D])
    pin = din("pin", [DEPTH, XW, 256])
    ck_in = din("ck", [DEPTH, 2, 128, 256])
    cv_in = din("cv", [DEPTH, 2, 128, 256])
    sp_in = din("sp", [DEPTH, 2, 15, 1024])
    c_ident = din("c_ident", [128, 128])
    c_R = din("c_R", [128, 128])
    c_cos = din("c_cos", [128, TK])
    c_sin = din("c_sin", [128, TK])
    c_kbias = din("c_kbias", [128, 24])
    c_invcnt = din("c_invcnt", [128, 8, 16])
    norm_mix = din("norm_mix", [DEPTH, D])
    w_in = din("w_in", [DEPTH, D, IN_W])
    q_norm = din("q_norm", [DEPTH, 64])
    k_norm = din("k_norm", [DEPTH, 64])
    sinks = din("sinks", [DEPTH, 16])
    pool_w = din("pool_w", [DEPTH, 4, 256, 256])
    pool_scale = din("pool_scale", [DEPTH, 1024])
    w_attn_br = din("w_attn_br", [DEPTH, 1024, D])
    w_pool_br = din("w_pool_br", [DEPTH, 1024, D])
    w_out = din("w_out", [DEPTH, D, D])
    norm_ffn = din("norm_ffn", [DEPTH, D])
    ffn_w_gate_up = din("ffn_w_gate_up", [1, D, 2 * DFF])
    ffn_w_down = din("ffn_w_down", [1, DFF, D])
    moe_router = din("moe_router", [1, D, NEXP])
    if tiny_moe:
        moe_w_gate_up = din("moe_w_gate_up", [1, NEXP, 8, 8])
        moe_w_down = din("moe_w_down", [1, NEXP, 8, 8])
    else:
        moe_w_gate_up = din("moe_w_gate_up", [1, NEXP, D, 2 * DFF])
        moe_w_down = din("moe_w_down", [1, NEXP, DFF, D])
    norm_ple = din("norm_ple", [DEPTH, D])
    w_ple = din("w_ple", [DEPTH, 256, D])
    w_ple_gate = din("w_ple_gate", [DEPTH, D, D])
    y_out = dout("y", [1152, D])
    nkp = dout("nkp", [DEPTH, 128, 256])
    nvp = dout("nvp", [DEPTH, 128, 256])
    npp = dout("npp", [DEPTH, 15, 1024])
    nks = dout("nks", [DEPTH, 2, 128, 256])
    nvs = dout("nvs", [DEPTH, 2, 128, 256])
    nps = dout("nps", [DEPTH, 2, 15, 1024])
    xs = nc.dram_tensor("xspill", [128, NCH * XW], F32, kind="Internal").ap()

    es.enter_context(nc.allow_low_precision("bf16 matmul operands with fp32 accumulation"))
    es.enter_context(nc.allow_non_contiguous_dma("small per-partition vector loads"))
    P = Prog(nc, es, debug)
    PE, ACT, DVE, POOL, SP = P.PE, P.ACT, P.DVE, P.POOL, P.SP

    def sb(name, shape, dt):
        return es.enter_context(nc.sbuf_tensor(name, list(shape), dt))

    arena = sb("arena", [128, NCH * XW], F32)
    Hb = sb("Hb", [128, NCH * TK], BF16)
    ring = sb("ring", [128, 4, 4096], BF16)
    bigst = sb("bigst", [128, 2, 1024], F32)
    tmp = sb("tmp", [128, 8, 512], F32)
    actbuf = sb("actbuf", [128, 2, 2, XW], BF16)
    sqb = sb("sqb", [128, 2, 512], BF16)
    ident = sb("ident", [128, 128], F32)
    Rm = sb("Rm", [128, 128], F32)
    Rg = sb("Rg", [128, 2, 128], F32)
    BD = sb("BD", [128, 128], F32)
    ones_f = sb("ones_f", [128, 128], F32)
    ones_b = sb("ones_b", [128, 128], BF16)
    onesz = sb("onesz", [128, 192], BF16)
    gam = sb("gam", [128, 3, 16], F32)
    pscale = sb("pscale", [128, 8], F32)
    qkg = sb("qkg", [128, 2], F32)
    esink = sb("esink", [128, 8], F32)
    sk16 = sb("sk16", [128, 16], F32)
    kbias = sb("kbias", [128, 24], F32)
    invcnt = sb("invcnt", [128, 8, 16], F32)
    histT = sb("histT", [128, 8, 2, 16], F32)
    kTc = sb("kTc", [128, 4, 2, 128], BF16)
    pTa = sb("pTa", [128, 4, 128], BF16)
    pTb = sb("pTb", [128, 4, 128], BF16)
    dent = sb("dent", [128, 4, 64], F32)
    qbd = sb("qbd", [128, 4, 128], BF16)
    pst = sb("pst", [16, 1, 3, 128], F32)
    rtr = sb("rtr", [128, 16, 8], F32)
    lg = sb("lg", [128, 9, 8], F32)
    comb = sb("comb", [128, 9, 8], F32)
    rsm = sb("rsm", [128, 9, 8], F32)
    psb = [es.enter_context(nc.psum_tensor("ps%d" % i, [128, 512], F32)) for i in range(8)]

    xv = arena[:, :].rearrange("p (k c) -> p k c", k=NCH)

    def xap(k, c0, c1):
        if c1 <= 128:
            return tmp[:, 0:4, :].rearrange("p a b -> p (a b)").rearrange("p (k c) -> p k c", k=NCH)[:, k, c0:c1]
        assert c0 >= 128
        return xv[:, k, c0 - 128:c1 - 128]

    def xck(k, c0, c1):
        if c1 <= 128:
            return [("tmp", s) for s in range(4)]
        return cks("x", k, c0, c1)

    hv = Hb[:, :].rearrange("p (k c) -> p k c", k=NCH)
    xlo = Hb[:, 0:2 * 8 * XW].bitcast(F32).rearrange("p (k c) -> p k c", k=8)
    A_bf = arena[:, 0:10240].bitcast(BF16)
    kT = A_bf[:, 0:4 * TK].rearrange("p (g c) -> p g c", g=4)
    Vze = A_bf[:, 5632:5632 + 11 * 768].rearrange("p (b g d) -> p b g d", b=11, g=4)
    cosT = arena[:, 7040:7040 + TK]
    sinT = arena[:, 8448:8448 + TK]
    m_t = A_bf[:, 0:NCH * XW].rearrange("p (k c) -> p k c", k=NCH)
    ub3 = arena[:, 0:3 * 1536].rearrange("p (a c) -> p a c", a=3)
    qo = arena[:, 10240:15360].bitcast(BF16).rearrange("p (k c) -> p k c", k=8)
    dp = arena[:, 15360:20480].bitcast(BF16).rearrange("p (k c) -> p k c", k=8)
    Vzo = arena[:, 15360:15360 + 11 * 384].bitcast(BF16).rearrange("p (b g d) -> p b g d", b=11, g=4)

    Vzc = arena[:, 15360 + 4224:15360 + 4224 + 768].bitcast(BF16).rearrange("p (s g d) -> p s g d", s=2, g=4)

    kf = arena[:, 10240:10240 + 1024].rearrange("p (g c) -> p g c", g=4)

    def qoap(j, c0, c1):
        return qo[:, j, c0 - 128:c1 - 128]

    def dpap(j, c0, c1):
        return dp[:, j, c0 - 128:c1 - 128]

    def map_(k, c0, c1):
        return m_t[:, k, c0 - 128:c1 - 128]

    s_init = P.dsem("d_init")
    s_lay = P.dsem("d_lay")
    s_big = [P.dsem("d_big0"), P.dsem("d_big1")]
    s_ring = [P.dsem("d_ring%d" % i) for i in range(4)]
    for d in s_ring:
        d.ring = True
    s_spill = P.dsem("d_spill")
    s_xr = [P.dsem("d_xr%d" % i) for i in range(8)]
    s_out = P.dsem("d_out")
    s_pst = [P.dsem("d_pst0"), P.dsem("d_pst1")]
    s_tst = [P.dsem("d_tst%d" % i) for i in range(8)]

    def bank():
        b = P.bank_i
        P.bank_i = (b + 1) % 8
        return b

    def tslot():
        s = P.tmp_i
        P.tmp_i = (s + 1) % 8
        return s

    ring_i = [0]

    def wload(parts):
        s = ring_i[0]
        ring_i[0] = (s + 1) % 4
        key = ("ring", s)
        slot = ring[:, s, :]
        for dst_fn, src in parts:
            P.dma(POOL, dst_fn(slot), src, s_ring[s], (), [key])
        return slot, key

    def w_k16(Wl, col0):
        slot, key = wload([(lambda s: s.rearrange("p (a b) -> p a b", a=16),
                            Wl[:, col0:col0 + 256].rearrange("(kc p) n -> p kc n", p=128))])
        return slot.rearrange("p (a b) -> p a b", a=16), key

    dbg_list = []

    def dbg(name, ap, shape, dt, reads):
        if name not in debug:
            return
        o = dout("dbg_" + name, shape, dt)
        P.dma(SP, o, ap, s_out, reads, (), commit_reads=False)
        P.barrier()
        dbg_list.append(name)

    P.dma(SP, ident[:, :], c_ident, s_init, (), [("ident",)])
    P.dma(SP, Rm[:, :], c_R, s_init, (), [("Rm",)])
    P.dma(SP, kbias[:, :], c_kbias, s_init, (), [("kbias",)])
    P.dma(SP, invcnt[:, :, :], c_invcnt, s_init, (), [("invcnt",)])
    P.group_commit(s_init, [("ident",), ("Rm",), ("kbias",), ("invcnt",)])
    P.memset(DVE, BD[:, :], 0.0, [("BD",)])
    P.memset(DVE, BD[0:64, 0:64], 1.0, [("BD",)])
    P.memset(DVE, BD[64:128, 64:128], 1.0, [("BD",)])
    P.memset(DVE, ones_f[:, :], 1.0, [("ones_f",)])
    P.memset(DVE, ones_b[:, :], 1.0, [("ones_b",)])
    P.memset(DVE, onesz[:, :], 0.0, [("onesz",)])
    P.memset(DVE, onesz[:, 64:128], 1.0, [("onesz",)])
    P.memset(DVE, histT[:, :, :, :], 0.0, [("histT", 0), ("histT", 1)])
    P.memset(DVE, qbd[:, :, :], 0.0, [("qbd", r_) for r_ in range(4)])
    P.memset(DVE, pTb[:, :, :], 0.0, [("pTb", r_) for r_ in range(4)])

    def rmsnorm(l, which, gvec, rng_blocks, want_f32=None):
        for (c0, c1) in rng_blocks:
            w = c1 - c0
            b = bank()
            P.job([("ps", b)])
            for k in range(NCH):
                s = k % 2
                P.act(sqb[:, s, 0:w], xap(k, c0, c1), AF.Square, xck(k, c0, c1), [("sqb", s)])
                P.mm(psb[b][:, 0:w], ones_b[:, :], sqb[:, s, 0:w], k == 0, k == NCH - 1,
                     [("ones_b",), ("sqb", s)], last=(k == NCH - 1), inc=True)
            ts_ = tslot()
            while ts_ < 4 and c1 <= 128:
                ts_ = tslot()
            if c1 <= 128:
                ts_ = 4
            rs = tmp[:, ts_, 0:w]
            P.act(rs, psb[b][:, 0:w], AF.Sqrt, [("ps", b)], [("tmp", ts_)], bias=EPS, scale=1.0 / D)
            P.op(DVE, lambda e, rs=rs: e.reciprocal(out=rs, in_=rs), [("tmp", ts_)], [("tmp", ts_)])
            for k in range(NCH):
                P.stt(hv[:, k, c0:c1], xap(k, c0, c1), gam[:, which, k:k + 1], rs, ALU.mult, ALU.mult,
                      xck(k, c0, c1) + [("tmp", ts_), ("gam", l, which)], cks("h", k, c0, c1))
            if want_f32 is not None:
                want_f32(c0, c1, rs, ("tmp", ts_))

    def load_vec16(dst, src_row, key):
        P.dma(SP, dst, src_row.rearrange("(kc p) -> p kc", p=128), s_lay, (), [key])


    def qk_chain(b, w, c0, c1, which, out_bf, out_bf_keys, out_f=None, out_f_keys=()):
        t_qs, t_sq, t_rs, t_t1 = tslot(), tslot(), tslot(), tslot()
        qs, sq, rs, t1 = (tmp[:, t, 0:w] for t in (t_qs, t_sq, t_rs, t_t1))
        P.copy(ACT, qs, psb[b][:, 0:w], [("ps", b)], [("tmp", t_qs)])
        P.act(sq, psb[b][:, 0:w], AF.Square, [("ps", b)], [("tmp", t_sq)])
        b2 = bank()
        P.job([("ps", b2)])
        P.mm(psb[b2][:, 0:w], BD[:, :], sq, True, True, [("BD",), ("tmp", t_sq)], last=True)
        b3 = bank()
        P.job([("ps", b3)])
        P.mm(psb[b3][:, 0:w], Rg[:, which, :], qs, True, True, [("Rg",), ("tmp", t_qs)], last=True)
        P.act(rs, psb[b2][:, 0:w], AF.Sqrt, [("ps", b2)], [("tmp", t_rs)], bias=EPS, scale=1.0 / 64)
        P.op(DVE, lambda e: e.reciprocal(out=rs, in_=rs), [("tmp", t_rs)], [("tmp", t_rs)])
        P.stt(t1, qs, qkg[:, which:which + 1], cosT[:, c0:c1], ALU.mult, ALU.mult,
              [("tmp", t_qs), ("qkg",), ("cos",)], [("tmp", t_t1)])
        P.tt(sq, psb[b3][:, 0:w], sinT[:, c0:c1], ALU.mult, [("ps", b3), ("sin",)], [("tmp", t_sq)])
        P.tt(t1, t1, sq, ALU.add, [("tmp", t_t1), ("tmp", t_sq)], [("tmp", t_t1)])
        P.tt(out_bf, t1, rs, ALU.mult, [("tmp", t_t1), ("tmp", t_rs)], out_bf_keys)
        if out_f is not None:
            o0 = max(c0, 1152)
            P.tt(out_f(o0, c1), t1[:, o0 - c0:w], rs[:, o0 - c0:w], ALU.mult, [("tmp", t_t1), ("tmp", t_rs)], out_f_keys)

    def layer(l):
        F0, F1 = FR[l]
        K0, K1 = KVR[l]
        fblocks = blocks(F0, F1)
        if l == 0:
            kvblocks = [(0, 128)] + blocks(128, TK)
        else:
            kvblocks = blocks(K0, K1)
        Wl = w_in[l]

        load_vec16(gam[:, 0, :], norm_mix[l], ("gam", l, 0))
        load_vec16(gam[:, 1, :], norm_ffn[l], ("gam", l, 1))
        load_vec16(gam[:, 2, :], norm_ple[l], ("gam", l, 2))
        P.dma(SP, pscale[:, :], pool_scale[l].rearrange("(kc p) -> p kc", p=128), s_lay, (), [("pscale",)])
        for hh in range(2):
            P.dma(SP, qkg[hh * 64:(hh + 1) * 64, 0:1], q_norm[l].rearrange("(p o) -> p o", o=1), s_lay, (), [("qkg",)])
            P.dma(SP, qkg[hh * 64:(hh + 1) * 64, 1:2], k_norm[l].rearrange("(p o) -> p o", o=1), s_lay, (), [("qkg",)])
        P.dma(SP, sk16[:, :], sinks[l:l + 1, :].to_broadcast([128, 16]), s_lay, (), [("sk16",)])
        P.group_commit(s_lay, [("gam", l, 0), ("gam", l, 1), ("gam", l, 2), ("pscale",), ("qkg",), ("sk16",)])
        for hh in range(2):
            P.act(esink[hh * 64:(hh + 1) * 64, :], sk16[hh * 64:(hh + 1) * 64, :].rearrange("p (j two) -> p j two", two=2)[:, :, hh],
                  AF.Exp, [("sk16",)], [("esink",)])
        for which in range(2):
            P.ts(Rg[:, which, :], Rm[:, :], qkg[:, which:which + 1], ALU.mult, [("Rm",), ("qkg",)], [("Rg",)])

        if l == 0:
            for rb in range(11):
                for half in range(2):
                    s = half
                    P.dma(SP, bigst[:, s, :], xin[rb * 128:(rb + 1) * 128, half * 1024:(half + 1) * 1024], s_big[s],
                          (), [("bigst", s)])
                    for jj in range(2):
                        b = bank()
                        P.job([("ps", b)])
                        for kk in range(4):
                            P.mm(psb[b][:, kk * 128:(kk + 1) * 128], bigst[:, s, (jj * 4 + kk) * 128:(jj * 4 + kk + 1) * 128],
                                 ident[:, :], True, True, [("bigst", s), ("ident",)], last=(kk == 3))
                        k0 = half * 8 + jj * 4
                        src = psb[b][:, :].rearrange("p (a c) -> p a c", a=4)
                        if rb == 0:
                            dst = tmp[:, 0:4, :].rearrange("p a b -> p (a b)").rearrange("p (k c) -> p k c", k=NCH)[:, k0:k0 + 4, :]
                            wk = [("tmp", t) for t in range(4)]
                        else:
                            dst = xv[:, k0:k0 + 4, (rb - 1) * 128:rb * 128]
                            wk = [("x", k0 + i, rb) for i in range(4)]
                        P.copy(P.alt(), dst, src, [("ps", b)], wk)
            P.tmp_i = 4

        rmsnorm(l, 0, None, kvblocks)
        dbg("h%d" % l, Hb[:, :], [128, NCH * TK], BF16, [("h", k, cb) for k in range(NCH) for cb in range(11)])
        for k in range(NCH):
            P.dma(SP, xs[:, k * XW:(k + 1) * XW], xv[:, k, :], s_spill, cks("x", k, 128, TK), [("xs", k)])
        P.barrier()
        if stop_after == "h":
            return False

        P.dma(SP, cosT, c_cos, s_lay, (), [("cos",)])
        P.dma(SP, sinT, c_sin, s_lay, (), [("sin",)])
        P.group_commit(s_lay, [("cos",), ("sin",)])
        P.memset(DVE, Vze[:, :, :, :], 0.0, [("Vze", cb) for cb in range(11)])
        P.memset(DVE, Vzo[:, :, :, :], 0.0, [("Vzo", cb) for cb in range(11)])
        P.memset(DVE, Vzc[:, :, :, :], 0.0, [("Vzc",)])
        for s in range(2):
            st = bigst[:, s, 0:512].rearrange("p (g t d) -> p g t d", g=4, t=2)
            for t in range(2):
                P.dma(SP, st[:, :, t, :], ck_in[l, s].rearrange("k (g d) -> k g d", g=4), s_big[s], (), [("bigst", s)])
            b = bank()
            P.job([("ps", b)])
            for g in range(4):
                P.mm(psb[b][:, g * 128:(g + 1) * 128], bigst[:, s, g * 128:(g + 1) * 128], ident[:, :], True, True,
                     [("bigst", s), ("ident",)], last=(g == 3))
            P.copy(DVE, kTc[:, :, s, :], psb[b][:, :].rearrange("p (g k) -> p g k", g=4), [("ps", b)], [("kTc", s)])
            P.dma(SP, bigst[:, s, 0:256], cv_in[l, s], s_big[s], (), [("bigst", s)])
            P.copy(DVE, Vzc[:, s, :, 64:128], bigst[:, s, 0:256].rearrange("p (g d) -> p g d", g=4),
                   [("bigst", s)], [("Vzc",)])
            P.dma(SP, nks[l, s, 0:64, :], ck_in[l, s, 64:128, :], s_out, (), ())
            P.dma(SP, nvs[l, s, 0:64, :], cv_in[l, s, 64:128, :], s_out, (), ())

        if stop_after == "kvpre":
            return False
        for t in range(2):
            def dstf(gg_, dupi):
                return lambda s_: s_.rearrange("p (kc gg u d) -> p kc gg u d", kc=16, gg=2, u=2)[:, :, gg_, dupi, :]

            def srcf(gg_):
                c_ = C_K + 128 * t + 64 * gg_
                return Wl[:, c_:c_ + 64].rearrange("(kc p) d -> p kc d", p=128)
            slot, wkey = wload([(dstf(gg_, du), srcf(gg_)) for gg_ in range(2) for du in range(2)])
            wt = slot.rearrange("p (kc n) -> p kc n", kc=16)
            for gg in range(2):
                g = 2 * t + gg
                for (c0, c1) in blocks(K0, K1):
                    w = c1 - c0
                    b = bank()
                    P.job([("ps", b)])
                    for k in range(NCH):
                        P.mm(psb[b][:, 0:w], wt[:, k, gg * 128:(gg + 1) * 128], hv[:, k, c0:c1], k == 0, k == NCH - 1,
                             [wkey] + cks("h", k, c0, c1), last=(k == NCH - 1))
                    outf = (lambda o0, o1, g=g: kf[:, g, o0 - 1152:o1 - 1152]) if c1 > 1152 else None
                    qk_chain(b, w, c0, c1, 1, kT[:, g, c0:c1], cks("kT", g, c0, c1), outf, [("kf", g)])
        if stop_after == "k":
            return False
        wv, wvkey = w_k16(Wl, C_V)
        for grid in range(2):
            for cb in range(11):
                t0 = cb * 128 + 64 * grid
                t1 = min(t0 + 128, TK)
                if t0 < K0 or t0 >= TK:
                    continue
                nt = t1 - t0
                b = bank()
                P.job([("ps", b)])
                for k in range(NCH):
                    P.mm(psb[b][0:nt, 0:256], hv[:, k, t0:t1], wv[:, k, :], k == 0, k == NCH - 1,
                         [wvkey] + cks("h", k, t0, t1), last=(k == NCH - 1))
                Vz = Vze if grid == 0 else Vzo
                P.copy(DVE, Vz[0:nt, cb, :, 64:128], psb[b][0:nt, 0:256].rearrange("p (g d) -> p g d", g=4),
                       [("ps", b)], [("Vze" if grid == 0 else "Vzo", cb)])
                if grid == 0 and cb in (9, 10):
                    ts_ = tslot()
                    P.copy(DVE, tmp[:, ts_, 0:256], psb[b][:, 0:256], [("ps", b)], [("tmp", ts_)])
                    if cb == 9:
                        P.dma(SP, nvp[l], tmp[:, ts_, 0:256], s_tst[ts_], [("tmp", ts_)], ())
                    else:
                        for s in range(2):
                            P.dma(SP, nvs[l, s, 64:128, :], tmp[64 * s:64 * s + 64, ts_, 0:256], s_tst[ts_], [("tmp", ts_)], ())
        if stop_after == "v":
            return False
        for part in range(2):
            b = bank()
            P.job([("ps", b)])
            for g in range(4):
                P.mm(psb[b][:, g * 64:(g + 1) * 64], kf[0:64, g, part * 128:(part + 1) * 128], ident[0:64, 0:64], True, True,
                     [("kf", g), ("ident",)], last=(g == 3))
            ts_ = tslot()
            P.copy(DVE, tmp[:, ts_, 0:256], psb[b][:, 0:256], [("ps", b)], [("tmp", ts_)])
            if part == 0:
                P.dma(SP, nkp[l], tmp[:, ts_, 0:256], s_tst[ts_], [("tmp", ts_)], ())
            else:
                for s in range(2):
                    P.dma(SP, nks[l, s, 64:128, :], tmp[64 * s:64 * s + 64, ts_, 0:256], s_tst[ts_], [("tmp", ts_)], ())

        for t in range(4):
            wt, wkey = w_k16(Wl, C_Q + 256 * t)
            for jj in range(2):
                j = 2 * t + jj
                for (c0, c1) in fblocks:
                    w = c1 - c0
                    b = bank()
                    P.job([("ps", b)])
                    for k in range(NCH):
                        P.mm(psb[b][:, 0:w], wt[:, k, jj * 128:(jj + 1) * 128], hv[:, k, c0:c1], k == 0, k == NCH - 1,
                             [wkey] + cks("h", k, c0, c1), last=(k == NCH - 1))
                    qk_chain(b, w, c0, c1, 0, qoap(j, c0, c1), cks("qo", j, c0, c1))
        dbg("q%d" % l, arena[:, 10240:15360], [128, 5120], F32, [("qo", j, cb) for j in range(8) for cb in range(11)])
        dbg("kT%d" % l, arena[:, 0:2816], [128, 2816], F32, [("kT", g, cb) for g in range(4) for cb in range(11)])
        if stop_after == "q":
            return False

        units = []
        for c in range(F0 // 64, 20):
            if c % 2 == 0:
                VA, VB = (Vze, (c - 2) // 2, "Vze"), (Vze, c // 2, "Vze")
            else:
                VA, VB = (Vzo, (c - 3) // 2, "Vzo"), (Vzo, (c - 1) // 2, "Vzo")
            units.append(dict(q0=64 * c, kA=lambda g, c=c: kT[:, g, 64 * (c - 2):64 * c], kAk=lambda g, c=c: cks("kT", g, 64 * (c - 2), 64 * c),
                              VA=VA, bA=c - 2, kB0=64 * c, VB=VB, bB=c))
        for s in range(2):
            VB = (Vze, 10, "Vze") if s == 0 else (Vzo, 10, "Vzo")
            units.append(dict(q0=C_S[s], kA=lambda g, s=s: kTc[:, g, s, :], kAk=lambda g, s=s: [("kTc", s)],
                              VA=(None, s, "Vzc"), bA=23, kB0=C_S[s], VB=VB, bB=23))
        items = [(u, j) for u in units for j in range(8)]

        def stage_a(i):
            u, j = items[i]
            q0 = u["q0"]
            g = j // 2
            r = i % 4
            qkeys = cks("qo", j, q0, q0 + 64)
            b = bank()
            P.job([("ps", b)])
            kA = u["kA"](g)
            kB = kT[:, g, u["kB0"]:u["kB0"] + 64]
            kBk = cks("kT", g, u["kB0"], u["kB0"] + 64)
            qa = qoap(j, q0, q0 + 64)
            P.copy(DVE, qbd[0:64, r, 0:64], qa[0:64, :], qkeys, [("qbd", r)])
            P.copy(DVE, qbd[64:128, r, 64:128], qa[64:128, :], qkeys, [("qbd", r)])
            P.mm(psb[b][:, 0:128], kA, qbd[:, r, :], True, True, u["kAk"](g) + [("qbd", r)])
            P.mm(psb[b][0:64, 128:256], kB, qbd[:, r, :], True, True, kBk, last=True)
            P.act(pTa[:, r, :], psb[b][:, 0:128], AF.Exp, [("ps", b), ("kbias",)], [("pTa", r)],
                  bias=kbias[:, u["bA"]:u["bA"] + 1], scale=0.125)
            P.act(pTb[0:64, r, :], psb[b][0:64, 128:256], AF.Exp, [("ps", b), ("kbias",)], [("pTb", r)],
                  bias=kbias[0:64, u["bB"]:u["bB"] + 1], scale=0.125)

        def stage_b(i):
            u, j = items[i]
            q0 = u["q0"]
            g = j // 2
            r = i % 4
            qkeys = cks("qo", j, q0, q0 + 64)
            qa = qoap(j, q0, q0 + 64)
            VzA, ia, na = u["VA"]
            VzB, ib, nb = u["VB"]
            if VzA is None:
                vA = Vzc[:, ia, g, :]
                vAk = [("Vzc",)]
            else:
                vA = VzA[:, ia, g, :]
                vAk = [(na, ia)]
            vB = VzB[:, ib, g, :]
            vBk = [(nb, ib)]
            b2 = bank()
            P.job([("ps", b2)])
            P.mm(psb[b2][:, 0:64], vA[:, 64:192], pTa[:, r, 0:64], True, False, vAk + [("pTa", r)])
            P.mm(psb[b2][:, 0:64], vA[:, 0:128], pTa[:, r, 64:128], False, False)
            P.mm(psb[b2][:, 0:64], vB[:, 64:192], pTb[:, r, 0:64], False, False, vBk + [("pTb", r)])
            P.mm(psb[b2][:, 0:64], vB[:, 0:128], pTb[:, r, 64:128], False, True)
            P.mm(psb[b2][:, 64:128], onesz[:, 64:192], pTa[:, r, 0:64], True, False, [("onesz",)])
            P.mm(psb[b2][:, 64:128], onesz[:, 0:128], pTa[:, r, 64:128], False, False)
            P.mm(psb[b2][:, 64:128], onesz[:, 64:192], pTb[:, r, 0:64], False, False)
            P.mm(psb[b2][:, 64:128], onesz[:, 0:128], pTb[:, r, 64:128], False, True, last=True)
            dn = dent[:, r, :]
            P.ts(dn, psb[b2][:, 64:128], esink[:, j:j + 1], ALU.add, [("ps", b2), ("esink",)], [("dent", r)])
            P.op(DVE, lambda e, dn=dn: e.reciprocal(out=dn, in_=dn), [("dent", r)], [("dent", r)])
            P.tt(qa, psb[b2][:, 0:64], dn, ALU.mult, [("ps", b2), ("dent", r)], qkeys)

        stage_a(0)
        for i in range(1, len(items)):
            stage_a(i)
            stage_b(i - 1)
        stage_b(len(items) - 1)
        dbg("o%d" % l, arena[:, 10240:15360], [128, 5120], F32, [("qo", j, cb) for j in range(8) for cb in range(11)])
        P.barrier()
        if stop_after == "attn":
            return False

        for s in range(2):
            P.dma(SP, bigst[0:15, s, :], sp_in[l, s], s_big[s], (), [("bigst", s)])
            b = bank()
            P.job([("ps", b)])
            for uc in range(8):
                P.mm(psb[b][:, uc * 16 + 1:uc * 16 + 16], bigst[0:15, s, uc * 128:(uc + 1) * 128], ident[0:15, 0:15], True, True,
                     [("bigst", s), ("ident",)], last=(uc == 7))
            P.copy(DVE, histT[:, :, s, 1:16], psb[b][:, 0:128].rearrange("p (u c) -> p u c", u=8)[:, :, 1:16],
                   [("ps", b)], [("histT", s)])
        ub, sA, sB = ub3[:, 0, :], ub3[:, 1, :], ub3[:, 2, :]
        UW = 1440

        def umap(c0, c1):
            out = []
            segs = ((0, 1280, 0), (1280, 1344, 1296), (1344, 1408, 1376))
            for (a0, a1, o) in segs:
                lo, hi = max(c0, a0), min(c1, a1)
                if lo < hi:
                    out.append((lo - c0, hi - lo, o + lo - a0))
            return out

        for t in range(4):
            wt, wkey = w_k16(Wl, C_U + 256 * t)
            for jj in range(2):
                uc = 2 * t + jj
                gi = uc // 2
                for (c0, c1) in blocks(K0, K1):
                    w = c1 - c0
                    b = bank()
                    P.job([("ps", b)])
                    for k in range(NCH):
                        P.mm(psb[b][:, 0:w], wt[:, k, jj * 128:(jj + 1) * 128], hv[:, k, c0:c1], k == 0, k == NCH - 1,
                             [wkey] + cks("h", k, c0, c1), last=(k == NCH - 1))
                    for (so, n, uo) in umap(c0, c1):
                        P.copy(ACT, ub[:, uo:uo + n], psb[b][:, so:so + n], [("ps", b)], [("ub",)])
                for s in range(2):
                    P.copy(DVE, ub[:, 1280 + 80 * s:1296 + 80 * s], histT[:, uc, s, :], [("histT", s)], [("ub",)])
                if l == 1:
                    P.memset(DVE, ub[:, 0:128], 0.0, [("ub",)])
                P.memset(DVE, ub[:, 1280:1281], 0.0, [("ub",)])
                P.memset(DVE, ub[:, 1360:1361], 0.0, [("ub",)])
                P.tt(sA[:, 1:UW], ub[:, 1:UW], ub[:, 0:UW - 1], ALU.add, [("ub",)], [("sA",)])
                fin, fk = sA, ("sA",)
                if gi >= 1:
                    P.tt(sB[:, 3:UW], sA[:, 3:UW], sA[:, 1:UW - 2], ALU.add, [("sA",)], [("sB",)])
                    fin, fk = sB, ("sB",)
                if gi >= 2:
                    P.tt(sA[:, 7:UW], sB[:, 7:UW], sB[:, 3:UW - 4], ALU.add, [("sB",)], [("sA",)])
                    fin, fk = sA, ("sA",)
                if gi >= 3:
                    P.tt(sB[:, 15:UW], sA[:, 15:UW], sA[:, 7:UW - 8], ALU.add, [("sA",)], [("sB",)])
                    fin, fk = sB, ("sB",)
                wnd = 2 ** (gi + 1)
                for (so, n, uo) in umap(F0, F1):
                    cc0 = F0 + so
                    P.stt(dpap(uc, cc0, cc0 + n), fin[:, uo:uo + n], 1.0 / wnd, ub[:, uo:uo + n], ALU.mult, ALU.subtract,
                          [fk, ("ub",)], cks("dp", uc, cc0, cc0 + n))
                ts_ = tslot()
                tf = tmp[:, ts_, 0:16]
                P.tt(tf, fin[:, C_MAIN:C_MAIN + 16], invcnt[:, uc, :], ALU.mult, [fk, ("invcnt",)], [("tmp", ts_)])
                P.tt(dpap(uc, C_MAIN, C_MAIN + 16), tf, ub[:, C_MAIN:C_MAIN + 16], ALU.subtract, [("tmp", ts_), ("ub",)],
                     cks("dp", uc, C_MAIN, C_MAIN + 16))
                b = bank()
                P.job([("ps", b)])
                offs = (1265, 1296 + 49, 1376 + 49)
                for i, o in enumerate(offs):
                    P.mm(psb[b][0:15, i * 128:(i + 1) * 128], ub[:, o:o + 15], ident[:, :], True, True, [("ub",), ("ident",)],
                         last=(i == 2))
                ps_ = 0
                P.copy(DVE, pst[0:15, ps_, :, :], psb[b][0:15, 0:384].rearrange("p (a c) -> p a c", a=3), [("ps", b)], [("pst", ps_)])
                P.dma(SP, npp[l][:, uc * 128:(uc + 1) * 128], pst[0:15, ps_, 0, :], s_pst[ps_], [("pst", ps_)], ())
                for s in range(2):
                    P.dma(SP, nps[l, s][:, uc * 128:(uc + 1) * 128], pst[0:15, ps_, 1 + s, :], s_pst[ps_], [("pst", ps_)], ())
        dbg("d%d" % l, arena[:, 15360:20480], [128, 5120], F32, [("dp", j, cb) for j in range(8) for cb in range(11)])

        slot, wkey = wload([(lambda s_: s_[:, 0:2048].rearrange("p (a b) -> p a b", a=8),
                             pool_w[l].rearrange("g (cc p) d -> p (g cc) d", p=128))])
        wt = slot[:, 0:2048].rearrange("p (a b) -> p a b", a=8)
        for gi in range(4):
            for (c0, c1) in fblocks:
                w = c1 - c0
                bs = []
                for dh in range(2):
                    b = bank()
                    bs.append(b)
                    P.job([("ps", b)])
                    for cc in range(2):
                        P.mm(psb[b][:, 0:w], wt[:, 2 * gi + cc, dh * 128:(dh + 1) * 128], dpap(2 * gi + cc, c0, c1), cc == 0, cc == 1,
                             [wkey] + cks("dp", 2 * gi + cc, c0, c1), last=(cc == 1))
                for dh in range(2):
                    P.ts(dpap(2 * gi + dh, c0, c1), psb[bs[dh]][:, 0:w], pscale[:, 2 * gi + dh:2 * gi + dh + 1], ALU.mult,
                         [("ps", bs[dh]), ("pscale",)], cks("dp", 2 * gi + dh, c0, c1))
        dbg("pl%d" % l, arena[:, 15360:20480], [128, 5120], F32, [("dp", j, cb) for j in range(8) for cb in range(11)])
        P.barrier()
        if stop_after == "pool":
            return False

        for pr in range(8):
            def half(i):
                return lambda s_: s_[:, 2048 * i:2048 * (i + 1)].rearrange("p (a b) -> p a b", a=8)
            slot, wbk = wload([(half(0), w_attn_br[l][:, pr * 256:(pr + 1) * 256].rearrange("(kc p) n -> p kc n", p=128)),
                               (half(1), w_pool_br[l][:, pr * 256:(pr + 1) * 256].rearrange("(kc p) n -> p kc n", p=128))])
            wab = slot[:, 0:2048].rearrange("p (a b) -> p a b", a=8)
            wpb = slot[:, 2048:4096].rearrange("p (a b) -> p a b", a=8)
            wga, wgak = w_k16(Wl, C_GA + 256 * pr)
            wgb, wgbk = w_k16(Wl, C_GB + 256 * pr)
            for jj in range(2):
                n = 2 * pr + jj
                for (c0, c1) in fblocks:
                    w = c1 - c0
                    ba, bga, bc, bgb = bank(), bank(), bank(), bank()
                    P.job([("ps", ba)])
                    for k in range(8):
                        P.mm(psb[ba][:, 0:w], wab[:, k, jj * 128:(jj + 1) * 128], qoap(k, c0, c1), k == 0, k == 7,
                             [wbk] + cks("qo", k, c0, c1), last=(k == 7))
                    P.job([("ps", bga)])
                    for k in range(NCH):
                        P.mm(psb[bga][:, 0:w], wga[:, k, jj * 128:(jj + 1) * 128], hv[:, k, c0:c1], k == 0, k == NCH - 1,
                             [wgak] + cks("h", k, c0, c1), last=(k == NCH - 1))
                    P.job([("ps", bc)])
                    for k in range(8):
                        P.mm(psb[bc][:, 0:w], wpb[:, k, jj * 128:(jj + 1) * 128], dpap(k, c0, c1), k == 0, k == 7,
                             [wbk] + cks("dp", k, c0, c1), last=(k == 7))
                    P.job([("ps", bgb)])
                    for k in range(NCH):
                        P.mm(psb[bgb][:, 0:w], wgb[:, k, jj * 128:(jj + 1) * 128], hv[:, k, c0:c1], k == 0, k == NCH - 1,
                             [wgbk] + cks("h", k, c0, c1), last=(k == NCH - 1))
                    t1_, t2_ = tslot(), tslot()
                    s1, s2 = tmp[:, t1_, 0:w], tmp[:, t2_, 0:w]
                    P.act(s1, psb[bga][:, 0:w], AF.Sigmoid, [("ps", bga)], [("tmp", t1_)])
                    P.act(s2, psb[bgb][:, 0:w], AF.Sigmoid, [("ps", bgb)], [("tmp", t2_)])
                    P.tt(s1, s1, psb[ba][:, 0:w], ALU.mult, [("tmp", t1_), ("ps", ba)], [("tmp", t1_)])
                    P.tt(s2, s2, psb[bc][:, 0:w], ALU.mult, [("tmp", t2_), ("ps", bc)], [("tmp", t2_)])
                    P.tt(map_(n, c0, c1), s1, s2, ALU.add, [("tmp", t1_), ("tmp", t2_)], cks("m", n, c0, c1))
        P.barrier()

        xr_i = 0
        for pr in range(8):
            wt, wkey = w_k16(w_out[l], 256 * pr)
            for jj in range(2):
                n = 2 * pr + jj
                for (c0, c1) in fblocks:
                    w = c1 - c0
                    b = bank()
                    P.job([("ps", b)])
                    for k in range(NCH):
                        P.mm(psb[b][:, 0:w], wt[:, k, jj * 128:(jj + 1) * 128], map_(k, c0, c1), k == 0, k == NCH - 1,
                             [wkey] + cks("m", k, c0, c1), last=(k == NCH - 1))
                    ts_ = xr_i % 8
                    xr_i += 1
                    P.dma(SP, tmp[:, ts_, 0:w], xs[:, n * XW + c0 - 128:n * XW + c1 - 128], s_xr[ts_], [("xs", n)], [("tmp", ts_)])
                    if n >= 8:
                        dst, dk = xv[:, n, c0 - 128:c1 - 128], cks("x", n, c0, c1)
                    else:
                        dst, dk = xlo[:, n, c0 - 128:c1 - 128], cks("xlo", n, c0, c1)
                    P.tt(dst, tmp[:, ts_, 0:w], psb[b][:, 0:w], ALU.add, [("tmp", ts_), ("ps", b)], dk)
        P.barrier()
        for n in range(8):
            for (c0, c1) in fblocks:
                P.copy(P.alt(), xv[:, n, c0 - 128:c1 - 128], xlo[:, n, c0 - 128:c1 - 128], cks("xlo", n, c0, c1), cks("x", n, c0, c1))
        P.barrier()
        dbg("x1_%d" % l, arena[:, :], [128, NCH * XW], F32, [("x", k, cb) for k in range(NCH) for cb in range(1, 11)])
        if stop_after == "mix":
            return False

        P.tmp_i = 0
        if l % 2 == 0:
            rmsnorm(l, 1, None, fblocks)
        else:
            P.dma(SP, rtr[:, :, :], moe_router[0].rearrange("(kc p) e -> p kc e", p=128), s_lay, (), [("rtr",)])
            state = {"n": 0}
            ntb = (F1 - F0) // 128

            def router_block(c0, c1, rs, rsk):
                for i in range((c1 - c0) // 128):
                    tb_ = (c0 - F0) // 128 + i
                    a0 = c0 + i * 128
                    lb = bank()
                    P.job([("ps", lb)])
                    for k in range(NCH):
                        ts2 = 6 + (state["n"] % 2)
                        state["n"] += 1
                        hf = tmp[:, ts2, 0:128]
                        P.stt(hf, xap(k, a0, a0 + 128), gam[:, 1, k:k + 1], rs[:, i * 128:(i + 1) * 128], ALU.mult, ALU.mult,
                              xck(k, a0, a0 + 128) + [rsk, ("gam", l, 1)], [("tmp", ts2)])
                        P.mm(psb[lb][:, 0:8], hf, rtr[:, k, :], k == 0, k == NCH - 1, [("tmp", ts2), ("rtr",)],
                             last=(k == NCH - 1), inc=True)
                    P.copy(DVE, lg[:, tb_, :], psb[lb][:, 0:8], [("ps", lb)], [("lg", tb_)])

            P.tmp_i = 0
            rmsnorm(l, 1, None, fblocks, want_f32=router_block)
            P.tmp_i = 0
            for tb_ in range(ntb):
                L = lg[:, tb_, :]
                sc = rsm[:, tb_, :]
                P.op(DVE, lambda e, L=L, sc=sc: e.tensor_reduce(out=sc[:, 0:1], in_=L, axis=AX.X, op=ALU.max), [("lg", tb_)], [("rsm", tb_)])
                P.ts(sc[:, 2:3], sc[:, 0:1], -1.0, ALU.mult, [("rsm", tb_)], [("rsm", tb_)])
                c_ = comb[:, tb_, :]
                P.ts(c_, L, sc[:, 0:1], ALU.is_equal, [("lg", tb_), ("rsm", tb_)], [("comb", tb_)], s2=-1e30, op1=ALU.mult)
                P.tt(c_, c_, L, ALU.add, [("comb", tb_), ("lg", tb_)], [("comb", tb_)])
                P.op(DVE, lambda e, c_=c_, sc=sc: e.tensor_reduce(out=sc[:, 1:2], in_=c_, axis=AX.X, op=ALU.max), [("comb", tb_)], [("rsm", tb_)])
                P.ts(c_, L, sc[:, 1:2], ALU.is_ge, [("lg", tb_), ("rsm", tb_)], [("comb", tb_)])
                ex = rsm[:, tb_, 3:4]
                P.act(lg[:, tb_, :], L, AF.Exp, [("lg", tb_), ("rsm", tb_)], [("lg", tb_)], bias=sc[:, 2:3], scale=1.0)
                P.tt(c_, c_, lg[:, tb_, :], ALU.mult, [("comb", tb_), ("lg", tb_)], [("comb", tb_)])
                P.op(DVE, lambda e, c_=c_, ex=ex: e.tensor_reduce(out=ex, in_=c_, axis=AX.X, op=ALU.add), [("comb", tb_)], [("rsm", tb_)])
                P.op(DVE, lambda e, ex=ex: e.reciprocal(out=ex, in_=ex), [("rsm", tb_)], [("rsm", tb_)])
                P.ts(c_, c_, ex, ALU.mult, [("comb", tb_), ("rsm", tb_)], [("comb", tb_)])
        dbg("h2_%d" % l, Hb[:, :], [128, NCH * TK], BF16, [("h", k, cb) for k in range(NCH) for cb in range(11)])
        if l % 2 == 1:
            dbg("comb", comb[:, :, :], [128, 9, 8], F32, [("comb", t) for t in range(9)])
        if stop_after == "h2":
            return False

        cmbt = tmp[:, 5:8, :].rearrange("p a b -> p (a b)")

        def ffn(Wgu, Wd, e):
            if e is not None:
                ntb = (F1 - F0) // 128
                for q4 in range(0, ntb, 4):
                    nb_ = min(4, ntb - q4)
                    b = bank()
                    P.job([("ps", b)])
                    for i in range(nb_):
                        tb_ = q4 + i
                        dg = tmp[:, i % 2, 0:128]
                        P.ts(dg, ident[:, :], comb[:, tb_, e:e + 1], ALU.mult, [("ident",), ("comb", tb_)], [("tmp", i % 2)])
                        P.mm(psb[b][:, i * 128:(i + 1) * 128], ones_f[:, :], dg, True, True, [("ones_f",), ("tmp", i % 2)],
                             last=(i == nb_ - 1), inc=True)
                    P.copy(ACT, cmbt[:, q4 * 128:(q4 + nb_) * 128], psb[b][:, 0:nb_ * 128], [("ps", b)], [("cmb",)])
            for grp in range(NHID // 2):
                j0 = 2 * grp
                par = grp % 2
                wg, wgk = w_k16(Wgu, j0 * 128)
                wu, wuk = w_k16(Wgu, DFF + j0 * 128)
                slot, wdk = wload([(lambda s_: s_.rearrange("p (a b) -> p a b", a=2),
                                    Wd[j0 * 128:(j0 + 2) * 128, :].rearrange("(jj p) n -> p jj n", p=128))])
                wd = slot.rearrange("p (a b) -> p a b", a=2)
                for jj in range(2):
                    for (c0, c1) in fblocks:
                        w = c1 - c0
                        bg, bu = bank(), bank()
                        P.job([("ps", bg)])
                        for k in range(NCH):
                            P.mm(psb[bg][:, 0:w], wg[:, k, jj * 128:(jj + 1) * 128], hv[:, k, c0:c1], k == 0, k == NCH - 1,
                                 [wgk] + cks("h", k, c0, c1), last=(k == NCH - 1))
                        P.job([("ps", bu)])
                        for k in range(NCH):
                            P.mm(psb[bu][:, 0:w], wu[:, k, jj * 128:(jj + 1) * 128], hv[:, k, c0:c1], k == 0, k == NCH - 1,
                                 [wuk] + cks("h", k, c0, c1), last=(k == NCH - 1))
                        ts_ = 2 + (P.tmp_i % 3)
                        P.tmp_i += 1
                        sg = tmp[:, ts_, 0:w]
                        P.act(sg, psb[bg][:, 0:w], AF.Silu, [("ps", bg)], [("tmp", ts_)])
                        if e is not None:
                            P.tt(sg, sg, cmbt[:, c0 - F0:c1 - F0], ALU.mult, [("tmp", ts_), ("cmb",)], [("tmp", ts_)])
                        P.tt(actbuf[:, par, jj, c0 - 128:c1 - 128], sg, psb[bu][:, 0:w], ALU.mult, [("tmp", ts_), ("ps", bu)],
                             cks("act%d" % par, jj, c0, c1))
                for n in range(NCH):
                    for (c0, c1) in fblocks:
                        w = c1 - c0
                        b = bank()
                        P.job([("ps", b)])
                        for jj in range(2):
                            P.mm(psb[b][:, 0:w], wd[:, jj, n * 128:(n + 1) * 128], actbuf[:, par, jj, c0 - 128:c1 - 128], jj == 0, jj == 1,
                                 [wdk] + cks("act%d" % par, jj, c0, c1), last=(jj == 1))
                        P.tt(xv[:, n, c0 - 128:c1 - 128], xv[:, n, c0 - 128:c1 - 128], psb[b][:, 0:w], ALU.add,
                             cks("x", n, c0, c1) + [("ps", b)], cks("x", n, c0, c1))

        if l % 2 == 0:
            ffn(ffn_w_gate_up[l // 2], ffn_w_down[l // 2], None)
        else:
            for e in range(NEXP):
                ffn(moe_w_gate_up[l // 2, e], moe_w_down[l // 2, e], e)
        dbg("x2_%d" % l, arena[:, :], [128, NCH * XW], F32, [("x", k, cb) for k in range(NCH) for cb in range(1, 11)])
        P.barrier()
        if stop_after == "ffn":
            return False

        P.tmp_i = 0
        rmsnorm(l, 2, None, fblocks)
        pT = actbuf[:, :, :, :].rearrange("p a b c -> p (a b c)")[:, 0:2 * XW].rearrange("p (k c) -> p k c", k=2)
        for rb in range((F0 - 128) // 128, 10):
            s = rb % 2
            P.dma(SP, bigst[:, s, 0:256], pin[l, rb * 128:(rb + 1) * 128, :], s_big[s], (), [("bigst", s)])
            b = bank()
            P.job([("ps", b)])
            for kc in range(2):
                P.mm(psb[b][:, kc * 128:(kc + 1) * 128], bigst[:, s, kc * 128:(kc + 1) * 128], ident[:, :], True, True,
                     [("bigst", s), ("ident",)], last=(kc == 1))
            P.copy(DVE, pT[:, :, rb * 128:(rb + 1) * 128], psb[b][:, 0:256].rearrange("p (k c) -> p k c", k=2),
                   [("ps", b)], [("pT", rb + 1)])
        for pr in range(8):
            slot, wpk = wload([(lambda s_: s_.rearrange("p (a b) -> p a b", a=2),
                                w_ple[l].rearrange("(kc p) n -> p kc n", p=128))])
            wp = slot.rearrange("p (a b) -> p a b", a=2)
            wt, wkey = w_k16(w_ple_gate[l], 256 * pr)
            for jj in range(2):
                n = 2 * pr + jj
                for (c0, c1) in fblocks:
                    w = c1 - c0
                    bg, bp = bank(), bank()
                    P.job([("ps", bg)])
                    for k in range(NCH):
                        P.mm(psb[bg][:, 0:w], wt[:, k, jj * 128:(jj + 1) * 128], hv[:, k, c0:c1], k == 0, k == NCH - 1,
                             [wkey] + cks("h", k, c0, c1), last=(k == NCH - 1))
                    P.job([("ps", bp)])
                    for kc in range(2):
                        P.mm(psb[bp][:, 0:w], wp[:, kc, n * 128:(n + 1) * 128], pT[:, kc, c0 - 128:c1 - 128], kc == 0, kc == 1,
                             [wpk] + [("pT", cb) for cb in range(c0 // 128, c1 // 128)], last=(kc == 1))
                    ts_ = tslot()
                    sg = tmp[:, ts_, 0:w]
                    P.act(sg, psb[bg][:, 0:w], AF.Sigmoid, [("ps", bg)], [("tmp", ts_)])
                    P.tt(sg, sg, psb[bp][:, 0:w], ALU.mult, [("tmp", ts_), ("ps", bp)], [("tmp", ts_)])
                    P.tt(xv[:, n, c0 - 128:c1 - 128], xv[:, n, c0 - 128:c1 - 128], sg, ALU.add,
                         cks("x", n, c0, c1) + [("tmp", ts_)], cks("x", n, c0, c1))
        dbg("x3_%d" % l, arena[:, :], [128, NCH * XW], F32, [("x", k, cb) for k in range(NCH) for cb in range(1, 11)])
        P.barrier()
        return True

    ok = True
    for l in range(nlayers):
        ok = layer(l)
        if not ok:
            break

    if ok:
        for rb in range(9):
            for half in range(2):
                s = half
                for jj in range(2):
                    b = bank()
                    P.job([("ps", b)])
                    for kk in range(4):
                        k = half * 8 + jj * 4 + kk
                        P.mm(psb[b][:, kk * 128:(kk + 1) * 128], xv[:, k, 128 + rb * 128:256 + rb * 128], ident[:, :], True, True,
                             [("x", k, rb + 2), ("ident",)], last=(kk == 3))
                    P.copy(P.alt(), bigst[:, s, jj * 512:(jj + 1) * 512], psb[b][:, :], [("ps", b)], [("bigst", s)])
                P.dma(SP, y_out[rb * 128:(rb + 1) * 128, half * 1024:(half + 1) * 1024], bigst[:, s, :], s_big[s], [("bigst", s)], ())

    for d in P.dsems:
        if d.issued > 0:
            P.need(SP, Tok(d.sem, d.issued))
    for E in (PE, ACT, DVE):
        if E.count > 0:
            P.need(SP, Tok(E.sem, E.count))

    def run_q(E):
        def f(e):
            for it in E.q:
                if it[0] == "w":
                    e.wait_ge(it[1], it[2])
                else:
                    ins = it[1](e)
                    if it[2] is not None:
                        ins.then_inc(it[2], it[3])
        return f

    with nc.Block() as block:
        block.tensor(run_q(PE))
        block.scalar(run_q(ACT))
        block.vector(run_q(DVE))
        block.gpsimd(run_q(POOL))
        block.sync(run_q(SP))
    es.close()
    stats = {E.name: (len(E.q), E.count) for E in P.engs}
    return nc, dbg_list, stats


def _consts(core):
    qd = core % 4
    s0 = qd * 1024
    pos = np.zeros(TK, np.float64)
    pos[:1280] = s0 - 256 + np.arange(1280)
    pos[1280:1344] = PAST + np.arange(64)
    pos[1344:1408] = PAST + np.arange(64)
    inv = (ROPE_THETA ** (-np.arange(0, 16, 2, dtype=np.float32) / 16)).astype(np.float32)
    ang = pos.astype(np.float32)[None, :] * inv[:, None]
    cos = np.ones((128, TK), np.float32)
    sin = np.zeros((128, TK), np.float32)
    for hh in range(2):
        for r in range(2):
            cos[hh * 64 + r * 8:hh * 64 + r * 8 + 8] = np.cos(ang)
            sin[hh * 64 + r * 8:hh * 64 + r * 8 + 8] = np.sin(ang)
    R = np.zeros((128, 128), np.float32)
    for hh in range(2):
        for i in range(8):
            R[hh * 64 + i + 8, hh * 64 + i] = -1.0
            R[hh * 64 + i, hh * 64 + i + 8] = 1.0
    kb = np.zeros((128, 24), np.float32)
    for cs in range(20):
        p = s0 - 256 + 64 * cs + np.arange(128)
        kb[:, cs] = np.where(p < 0, NEG, 0.0)
    ic = np.zeros((128, 8, 16), np.float32)
    for uc in range(8):
        w = 2 ** (uc // 2 + 1)
        p = s0 + np.arange(16)
        ic[:, uc, :] = (1.0 / np.minimum(w, p + 1))[None, :]
    return dict(c_ident=np.eye(128, dtype=np.float32), c_R=R, c_cos=cos, c_sin=sin, c_kbias=kb, c_invcnt=ic)


def _core_inputs(core, inp):
    b, qd = core // 4, core % 4
    s0 = qd * 1024
    m = {}
    xin = np.zeros((TK, D), np.float32)
    lo = s0 - 256
    a = max(lo, 0)
    xin[a - lo:1280] = inp["x_prompt"][b, a:s0 + 1024]
    xin[1280:1344] = inp["x_sample"][2 * core]
    xin[1344:1408] = inp["x_sample"][2 * core + 1]
    m["xin"] = xin
    pin = np.zeros((DEPTH, XW, 256), np.float32)
    lo = s0 - 128
    a = max(lo, 0)
    pin[:, a - lo:1152] = inp["p_prompt"][:, b, a:s0 + 1024]
    pin[:, 1152:1216] = inp["p_sample"][:, 2 * core]
    pin[:, 1216:1280] = inp["p_sample"][:, 2 * core + 1]
    m["pin"] = pin
    m["ck"] = np.ascontiguousarray(inp["cache_k"][:, 2 * core:2 * core + 2].reshape(DEPTH, 2, 128, 256))
    m["cv"] = np.ascontiguousarray(inp["cache_v"][:, 2 * core:2 * core + 2].reshape(DEPTH, 2, 128, 256))
    m["sp"] = np.ascontiguousarray(inp["state_pool"][:, 2 * core:2 * core + 2])
    m.update(_consts(core))
    return m


WEIGHTS = ["norm_mix", "w_in", "q_norm", "k_norm", "sinks", "pool_w", "pool_scale", "w_attn_br", "w_pool_br", "w_out",
           "norm_ffn", "ffn_w_gate_up", "ffn_w_down", "moe_router", "moe_w_gate_up", "moe_w_down", "norm_ple", "w_ple",
           "w_ple_gate"]


def kernel(**inputs):
    inp = {k: np.asarray(v) for k, v in inputs.items()}
    nc, _, _ = build_program()
    wts = {k: np.ascontiguousarray(inp[k], dtype=np.float32) for k in WEIGHTS}
    in_maps = []
    for c in range(NCORES):
        m = _core_inputs(c, inp)
        m.update(wts)
        in_maps.append(m)
    res = run_bass_kernel_spmd(nc, in_maps, core_ids=list(range(NCORES)))
    r = res.results
    y_prompt = np.zeros((2, 4096, D), np.float32)
    y_sample = np.zeros((16, 64, D), np.float32)
    nkp = np.zeros((DEPTH, 2, 128, 4, 64), np.float32)
    nvp = np.zeros((DEPTH, 2, 128, 4, 64), np.float32)
    npp = np.zeros((DEPTH, 2, 15, 1024), np.float32)
    nks = np.zeros((DEPTH, 16, 128, 4, 64), np.float32)
    nvs = np.zeros((DEPTH, 16, 128, 4, 64), np.float32)
    nps = np.zeros((DEPTH, 16, 15, 1024), np.float32)
    for c in range(NCORES):
        b, qd = c // 4, c % 4
        y_prompt[b, qd * 1024:(qd + 1) * 1024] = r[c]["y"][0:1024]
        y_sample[2 * c] = r[c]["y"][1024:1088]
        y_sample[2 * c + 1] = r[c]["y"][1088:1152]
        if qd == 3:
            nkp[:, b] = r[c]["nkp"].reshape(DEPTH, 128, 4, 64)
            nvp[:, b] = r[c]["nvp"].reshape(DEPTH, 128, 4, 64)
            npp[:, b] = r[c]["npp"]
        nks[:, 2 * c:2 * c + 2] = r[c]["nks"].reshape(DEPTH, 2, 128, 4, 64)
        nvs[:, 2 * c:2 * c + 2] = r[c]["nvs"].reshape(DEPTH, 2, 128, 4, 64)
        nps[:, 2 * c:2 * c + 2] = r[c]["nps"]
    return (y_prompt, y_sample, nkp, nvp, npp, nks, nvs, nps)
```

```python
import math
from contextlib import ExitStack

import numpy as np
import ml_dtypes
import concourse.bass as bass
import concourse.mybir as mybir
from concourse.bass_utils import run_bass_kernel_spmd

F32 = mybir.dt.float32
BF16 = mybir.dt.bfloat16
ALU = mybir.AluOpType
AF = mybir.ActivationFunctionType
AX = mybir.AxisListType

NCORES = 8
D = 2048
NCH = 16
DEPTH = 2
TK = 1408
XW = 1280
C_MAIN = 256
C_S = (1280, 1344)
FR = ((128, 1408), (256, 1408))
KVR = ((0, 1408), (128, 1408))
IN_W = 6656
C_Q, C_K, C_V, C_U, C_GA, C_GB = 0, 1024, 1280, 1536, 2560, 4608
DFF = 5632
NHID = 44
NEXP = 8
EPS = 1e-6
ROPE_THETA = 500000.0
PAST = 2048
NEG = -30000.0


def blocks(c0, c1, maxw=512):
    n = -(-(c1 - c0) // maxw)
    w = -(-((c1 - c0) // 128) // n) * 128
    out = []
    c = c0
    while c < c1:
        out.append((c, min(c + w, c1)))
        c += w
    return out


class Tok:
    __slots__ = ("sem", "val")

    def __init__(self, sem, val):
        self.sem = sem
        self.val = val


class Cell:
    __slots__ = ("w", "r")

    def __init__(self):
        self.w = None
        self.r = {}


class Eng:
    def __init__(self, name, sem):
        self.name = name
        self.sem = sem
        self.count = 0
        self.q = []
        self.waited = {}


class DSem:
    def __init__(self, sem):
        self.sem = sem
        self.issued = 0


class Prog:
    def __init__(self, nc, es, debug=None):
        self.nc = nc
        self.es = es
        self.cells = {}
        self.debug = debug or {}
        self.dbg_outs = {}
        mk = lambda n: es.enter_context(nc.semaphore(n))
        self.PE = Eng("pe", mk("s_pe"))
        self.ACT = Eng("act", mk("s_act"))
        self.DVE = Eng("dve", mk("s_dve"))
        self.POOL = Eng("pool", mk("s_pool"))
        self.SP = Eng("sp", mk("s_sp"))
        self.engs = [self.PE, self.ACT, self.DVE, self.POOL, self.SP]
        self.dsems = []
        self.bank_i = 0
        self.tmp_i = 0
        self.rr = 0

    def dsem(self, name):
        d = DSem(self.es.enter_context(self.nc.semaphore(name)))
        self.dsems.append(d)
        return d

    def cell(self, key):
        c = self.cells.get(key)
        if c is None:
            c = Cell()
            self.cells[key] = c
        return c

    def need(self, E, tok):
        if tok is None:
            return
        if E is self.PE and tok.sem is self.PE.sem:
            return
        k = id(tok.sem)
        if E.waited.get(k, 0) >= tok.val:
            return
        E.waited[k] = tok.val
        E.q.append(("w", tok.sem, tok.val))

    def _deps(self, E, reads, writes):
        for key in reads:
            self.need(E, self.cell(key).w)
        for key in writes:
            c = self.cell(key)
            self.need(E, c.w)
            for s, v in c.r.values():
                self.need(E, Tok(s, v))

    def _commit(self, tok, reads, writes):
        k = id(tok.sem)
        for key in reads:
            c = self.cell(key)
            old = c.r.get(k)
            if old is None or old[1] < tok.val:
                c.r[k] = (tok.sem, tok.val)
        for key in writes:
            c = self.cell(key)
            c.w = tok
            c.r = {}

    def op(self, E, fn, reads=(), writes=()):
        self._deps(E, reads, writes)
        E.count += 1
        tok = Tok(E.sem, E.count)
        E.q.append(("i", fn, E.sem, 1))
        self._commit(tok, reads, writes)
        return tok

    def dma(self, E, out, in_, ds, reads=(), writes=(), commit_reads=True):
        self._deps(E, reads, writes)
        ds.issued += 16
        tok = Tok(ds.sem, ds.issued)
        E.q.append(("i", lambda e: e.dma_start(out=out, in_=in_), ds.sem, 16))
        self._commit(tok, reads if commit_reads else (), writes)
        return tok

    def job(self, writes):
        self._deps(self.PE, (), writes)
        self.jwrites = list(writes)

    def mm(self, out, lhsT, rhs, start, stop, reads=(), last=False, inc=False):
        PE = self.PE
        for key in reads:
            self.need(PE, self.cell(key).w)
        tok = Tok(PE.sem, PE.count + 1)
        self._commit(tok, reads, ())
        if last or inc:
            PE.count += 1
            PE.q.append(("i", lambda e: e.matmul(out, lhsT=lhsT, rhs=rhs, start=start, stop=stop), PE.sem, 1))
            if last:
                self._commit(tok, (), self.jwrites)
        else:
            PE.q.append(("i", lambda e: e.matmul(out, lhsT=lhsT, rhs=rhs, start=start, stop=stop), None, 0))

    def group_commit(self, ds, keys):
        tok = Tok(ds.sem, ds.issued)
        for key in keys:
            self.cell(key).w = tok

    def barrier(self, engs=None):
        engs = engs or [self.PE, self.ACT, self.DVE, self.SP]
        for E in engs:
            for F in [self.PE, self.ACT, self.DVE]:
                if F.count > 0:
                    self.need(E, Tok(F.sem, F.count))
            for d in self.dsems:
                if d.issued > 0 and not getattr(d, "ring", False):
                    self.need(E, Tok(d.sem, d.issued))

    def act(self, out, in_, func, reads, writes, bias=None, scale=None):
        kw = {}
        if bias is not None:
            kw["bias"] = bias
        if scale is not None:
            kw["scale"] = scale
        return self.op(self.ACT, lambda e: e.activation(out=out, in_=in_, func=func, **kw), reads, writes)

    def tt(self, out, in0, in1, op, reads, writes, E=None):
        return self.op(E or self.DVE, lambda e: e.tensor_tensor(out=out, in0=in0, in1=in1, op=op), reads, writes)

    def ts(self, out, in0, s1, op0, reads, writes, s2=None, op1=None, E=None):
        if op1 is None:
            return self.op(E or self.DVE, lambda e: e.tensor_scalar(out=out, in0=in0, scalar1=s1, scalar2=None, op0=op0),
                           reads, writes)
        return self.op(E or self.DVE,
                       lambda e: e.tensor_scalar(out=out, in0=in0, scalar1=s1, scalar2=s2, op0=op0, op1=op1), reads, writes)

    def stt(self, out, in0, scalar, in1, op0, op1, reads, writes, E=None):
        return self.op(E or self.DVE,
                       lambda e: e.scalar_tensor_tensor(out=out, in0=in0, scalar=scalar, in1=in1, op0=op0, op1=op1),
                       reads, writes)

    def copy(self, E, out, in_, reads, writes):
        if E is self.ACT:
            return self.op(E, lambda e: e.copy(out=out, in_=in_), reads, writes)
        return self.op(E, lambda e: e.tensor_copy(out=out, in_=in_), reads, writes)

    def memset(self, E, ap, val, writes):
        return self.op(E, lambda e: e.memset(ap, val), (), writes)

    def alt(self):
        self.rr ^= 1
        return self.ACT if self.rr else self.DVE


def cks(name, chunk, c0, c1):
    return [(name, chunk, cb) for cb in range(c0 // 128, -(-c1 // 128))]


def build_program(debug=None, stop_after=None, nlayers=DEPTH, tiny_moe=False):
    debug = debug or {}
    nc = bass.Bass("TRN2", target_bir_lowering=False)
    es = ExitStack()

    def din(name, shape, dt=F32):
        return nc.dram_tensor(name, list(shape), dt, kind="ExternalInput").ap()

    def dout(name, shape, dt=F32):
        return nc.dram_tensor(name, list(shape), dt, kind="ExternalOutput").ap()

    xin = din("xin", [TK, D])
    pin = din("pin", [DEPTH, XW, 256])
    ck_in = din("ck", [DEPTH, 2, 128, 256])
    cv_in = din("cv", [DEPTH, 2, 128, 256])
    sp_in = din("sp", [DEPTH, 2, 15, 1024])
    c_ident = din("c_ident", [128, 128])
    c_R = din("c_R", [128, 128])
    c_cos = din("c_cos", [128, TK])
    c_sin = din("c_sin", [128, TK])
    c_kbias = din("c_kbias", [128, 24])
    c_invcnt = din("c_invcnt", [128, 8, 16])
    norm_mix = din("norm_mix", [DEPTH, D])
    w_in = din("w_in", [DEPTH, D, IN_W])
    q_norm = din("q_norm", [DEPTH, 64])
    k_norm = din("k_norm", [DEPTH, 64])
    sinks = din("sinks", [DEPTH, 16])
    pool_w = din("pool_w", [DEPTH, 4, 256, 256])
    pool_scale = din("pool_scale", [DEPTH, 1024])
    w_attn_br = din("w_attn_br", [DEPTH, 1024, D])
    w_pool_br = din("w_pool_br", [DEPTH, 1024, D])
    w_out = din("w_out", [DEPTH, D, D])
    norm_ffn = din("norm_ffn", [DEPTH, D])
    ffn_w_gate_up = din("ffn_w_gate_up", [1, D, 2 * DFF])
    ffn_w_down = din("ffn_w_down", [1, DFF, D])
    moe_router = din("moe_router", [1, D, NEXP])
    if tiny_moe:
        moe_w_gate_up = din("moe_w_gate_up", [1, NEXP, 8, 8])
        moe_w_down = din("moe_w_down", [1, NEXP, 8, 8])
    else:
        moe_w_gate_up = din("moe_w_gate_up", [1, NEXP, D, 2 * DFF])
        moe_w_down = din("moe_w_down", [1, NEXP, DFF, D])
    norm_ple = din("norm_ple", [DEPTH, D])
    w_ple = din("w_ple", [DEPTH, 256, D])
    w_ple_gate = din("w_ple_gate", [DEPTH, D, D])
    y_out = dout("y", [1152, D])
    nkp = dout("nkp", [DEPTH, 128, 256])
    nvp = dout("nvp", [DEPTH, 128, 256])
    npp = dout("npp", [DEPTH, 15, 1024])
    nks = dout("nks", [DEPTH, 2, 128, 256])
    nvs = dout("nvs", [DEPTH, 2, 128, 256])
    nps = dout("nps", [DEPTH, 2, 15, 1024])
    xs = nc.dram_tensor("xspill", [128, NCH * XW], F32, kind="Internal").ap()

    es.enter_context(nc.allow_low_precision("bf16 matmul operands with fp32 accumulation"))
    es.enter_context(nc.allow_non_contiguous_dma("small per-partition vector loads"))
    P = Prog(nc, es, debug)
    PE, ACT, DVE, POOL, SP = P.PE, P.ACT, P.DVE, P.POOL, P.SP

    def sb(name, shape, dt):
        return es.enter_context(nc.sbuf_tensor(name, list(shape), dt))

    arena = sb("arena", [128, NCH * XW], F32)
    Hb = sb("Hb", [128, NCH * TK], BF16)
    ring = sb("ring", [128, 4, 4096], BF16)
    bigst = sb("bigst", [128, 2, 1024], F32)
    tmp = sb("tmp", [128, 8, 512], F32)
    actbuf = sb("actbuf", [128, 2, 2, XW], BF16)
    sqb = sb("sqb", [128, 2, 512], BF16)
    ident = sb("ident", [128, 128], F32)
    Rm = sb("Rm", [128, 128], F32)
    Rg = sb("Rg", [128, 2, 128], F32)
    BD = sb("BD", [128, 128], F32)
    ones_f = sb("ones_f", [128, 128], F32)
    ones_b = sb("ones_b", [128, 128], BF16)
    onesz = sb("onesz", [128, 192], BF16)
    gam = sb("gam", [128, 3, 16], F32)
    pscale = sb("pscale", [128, 8], F32)
    qkg = sb("qkg", [128, 2], F32)
    esink = sb("esink", [128, 8], F32)
    sk16 = sb("sk16", [128, 16], F32)
    kbias = sb("kbias", [128, 24], F32)
    invcnt = sb("invcnt", [128, 8, 16], F32)
    histT = sb("histT", [128, 8, 2, 16], F32)
    kTc = sb("kTc", [128, 4, 2, 128], BF16)
    pTa = sb("pTa", [128, 4, 128], BF16)
    pTb = sb("pTb", [128, 4, 128], BF16)
    dent = sb("dent", [128, 4, 64], F32)
    qbd = sb("qbd", [128, 4, 128], BF16)
    pst = sb("pst", [16, 1, 3, 128], F32)
    rtr = sb("rtr", [128, 16, 8], F32)
    lg = sb("lg", [128, 9, 8], F32)
    comb = sb("comb", [128, 9, 8], F32)
    rsm = sb("rsm", [128, 9, 8], F32)
    psb = [es.enter_context(nc.psum_tensor("ps%d" % i, [128, 512], F32)) for i in range(8)]

    xv = arena[:, :].rearrange("p (k c) -> p k c", k=NCH)

    def xap(k, c0, c1):
        if c1 <= 128:
            return tmp[:, 0:4, :].rearrange("p a b -> p (a b)").rearrange("p (k c) -> p k c", k=NCH)[:, k, c0:c1]
        assert c0 >= 128
        return xv[:, k, c0 - 128:c1 - 128]

    def xck(k, c0, c1):
        if c1 <= 128:
            return [("tmp", s) for s in range(4)]
        return cks("x", k, c0, c1)

    hv = Hb[:, :].rearrange("p (k c) -> p k c", k=NCH)
    xlo = Hb[:, 0:2 * 8 * XW].bitcast(F32).rearrange("p (k c) -> p k c", k=8)
    A_bf = arena[:, 0:10240].bitcast(BF16)
    kT = A_bf[:, 0:4 * TK].rearrange("p (g c) -> p g c", g=4)
    Vze = A_bf[:, 5632:5632 + 11 * 768].rearrange("p (b g d) -> p b g d", b=11, g=4)
    cosT = arena[:, 7040:7040 + TK]
    sinT = arena[:, 8448:8448 + TK]
    m_t = A_bf[:, 0:NCH * XW].rearrange("p (k c) -> p k c", k=NCH)
    ub3 = arena[:, 0:3 * 1536].rearrange("p (a c) -> p a c", a=3)
    qo = arena[:, 10240:15360].bitcast(BF16).rearrange("p (k c) -> p k c", k=8)
    dp = arena[:, 15360:20480].bitcast(BF16).rearrange("p (k c) -> p k c", k=8)
    Vzo = arena[:, 15360:15360 + 11 * 384].bitcast(BF16).rearrange("p (b g d) -> p b g d", b=11, g=4)

    Vzc = arena[:, 15360 + 4224:15360 + 4224 + 768].bitcast(BF16).rearrange("p (s g d) -> p s g d", s=2, g=4)

    kf = arena[:, 10240:10240 + 1024].rearrange("p (g c) -> p g c", g=4)

    def qoap(j, c0, c1):
        return qo[:, j, c0 - 128:c1 - 128]

    def dpap(j, c0, c1):
        return dp[:, j, c0 - 128:c1 - 128]

    def map_(k, c0, c1):
        return m_t[:, k, c0 - 128:c1 - 128]

    s_init = P.dsem("d_init")
    s_lay = P.dsem("d_lay")
    s_big = [P.dsem("d_big0"), P.dsem("d_big1")]
    s_ring = [P.dsem("d_ring%d" % i) for i in range(4)]
    for d in s_ring:
        d.ring = True
    s_spill = P.dsem("d_spill")
    s_xr = [P.dsem("d_xr%d" % i) for i in range(8)]
    s_out = P.dsem("d_out")
    s_pst = [P.dsem("d_pst0"), P.dsem("d_pst1")]
    s_tst = [P.dsem("d_tst%d" % i) for i in range(8)]

    def bank():
        b = P.bank_i
        P.bank_i = (b + 1) % 8
        return b

    def tslot():
        s = P.tmp_i
        P.tmp_i = (s + 1) % 8
        return s

    ring_i = [0]

    def wload(parts):
        s = ring_i[0]
        ring_i[0] = (s + 1) % 4
        key = ("ring", s)
        slot = ring[:, s, :]
        for dst_fn, src in parts:
            P.dma(POOL, dst_fn(slot), src, s_ring[s], (), [key])
        return slot, key

    def w_k16(Wl, col0):
        slot, key = wload([(lambda s: s.rearrange("p (a b) -> p a b", a=16),
                            Wl[:, col0:col0 + 256].rearrange("(kc p) n -> p kc n", p=128))])
        return slot.rearrange("p (a b) -> p a b", a=16), key

    dbg_list = []

    def dbg(name, ap, shape, dt, reads):
        if name not in debug:
            return
        o = dout("dbg_" + name, shape, dt)
        P.dma(SP, o, ap, s_out, reads, (), commit_reads=False)
        P.barrier()
        dbg_list.append(name)

    P.dma(SP, ident[:, :], c_ident, s_init, (), [("ident",)])
    P.dma(SP, Rm[:, :], c_R, s_init, (), [("Rm",)])
    P.dma(SP, kbias[:, :], c_kbias, s_init, (), [("kbias",)])
    P.dma(SP, invcnt[:, :, :], c_invcnt, s_init, (), [("invcnt",)])
    P.group_commit(s_init, [("ident",), ("Rm",), ("kbias",), ("invcnt",)])
    P.memset(DVE, BD[:, :], 0.0, [("BD",)])
    P.memset(DVE, BD[0:64, 0:64], 1.0, [("BD",)])
    P.memset(DVE, BD[64:128, 64:128], 1.0, [("BD",)])
    P.memset(DVE, ones_f[:, :], 1.0, [("ones_f",)])
    P.memset(DVE, ones_b[:, :], 1.0, [("ones_b",)])
    P.memset(DVE, onesz[:, :], 0.0, [("onesz",)])
    P.memset(DVE, onesz[:, 64:128], 1.0, [("onesz",)])
    P.memset(DVE, histT[:, :, :, :], 0.0, [("histT", 0), ("histT", 1)])
    P.memset(DVE, qbd[:, :, :], 0.0, [("qbd", r_) for r_ in range(4)])
    P.memset(DVE, pTb[:, :, :], 0.0, [("pTb", r_) for r_ in range(4)])

    def rmsnorm(l, which, gvec, rng_blocks, want_f32=None):
        for (c0, c1) in rng_blocks:
            w = c1 - c0
            b = bank()
            P.job([("ps", b)])
            for k in range(NCH):
                s = k % 2
                P.act(sqb[:, s, 0:w], xap(k, c0, c1), AF.Square, xck(k, c0, c1), [("sqb", s)])
                P.mm(psb[b][:, 0:w], ones_b[:, :], sqb[:, s, 0:w], k == 0, k == NCH - 1,
                     [("ones_b",), ("sqb", s)], last=(k == NCH - 1), inc=True)
            ts_ = tslot()
            while ts_ < 4 and c1 <= 128:
                ts_ = tslot()
            if c1 <= 128:
                ts_ = 4
            rs = tmp[:, ts_, 0:w]
            P.act(rs, psb[b][:, 0:w], AF.Sqrt, [("ps", b)], [("tmp", ts_)], bias=EPS, scale=1.0 / D)
            P.op(DVE, lambda e, rs=rs: e.reciprocal(out=rs, in_=rs), [("tmp", ts_)], [("tmp", ts_)])
            for k in range(NCH):
                P.stt(hv[:, k, c0:c1], xap(k, c0, c1), gam[:, which, k:k + 1], rs, ALU.mult, ALU.mult,
                      xck(k, c0, c1) + [("tmp", ts_), ("gam", l, which)], cks("h", k, c0, c1))
            if want_f32 is not None:
                want_f32(c0, c1, rs, ("tmp", ts_))

    def load_vec16(dst, src_row, key):
        P.dma(SP, dst, src_row.rearrange("(kc p) -> p kc", p=128), s_lay, (), [key])


    def qk_chain(b, w, c0, c1, which, out_bf, out_bf_keys, out_f=None, out_f_keys=()):
        t_qs, t_sq, t_rs, t_t1 = tslot(), tslot(), tslot(), tslot()
        qs, sq, rs, t1 = (tmp[:, t, 0:w] for t in (t_qs, t_sq, t_rs, t_t1))
        P.copy(ACT, qs, psb[b][:, 0:w], [("ps", b)], [("tmp", t_qs)])
        P.act(sq, psb[b][:, 0:w], AF.Square, [("ps", b)], [("tmp", t_sq)])
        b2 = bank()
        P.job([("ps", b2)])
        P.mm(psb[b2][:, 0:w], BD[:, :], sq, True, True, [("BD",), ("tmp", t_sq)], last=True)
        b3 = bank()
        P.job([("ps", b3)])
        P.mm(psb[b3][:, 0:w], Rg[:, which, :], qs, True, True, [("Rg",), ("tmp", t_qs)], last=True)
        P.act(rs, psb[b2][:, 0:w], AF.Sqrt, [("ps", b2)], [("tmp", t_rs)], bias=EPS, scale=1.0 / 64)
        P.op(DVE, lambda e: e.reciprocal(out=rs, in_=rs), [("tmp", t_rs)], [("tmp", t_rs)])
        P.stt(t1, qs, qkg[:, which:which + 1], cosT[:, c0:c1], ALU.mult, ALU.mult,
              [("tmp", t_qs), ("qkg",), ("cos",)], [("tmp", t_t1)])
        P.tt(sq, psb[b3][:, 0:w], sinT[:, c0:c1], ALU.mult, [("ps", b3), ("sin",)], [("tmp", t_sq)])
        P.tt(t1, t1, sq, ALU.add, [("tmp", t_t1), ("tmp", t_sq)], [("tmp", t_t1)])
        P.tt(out_bf, t1, rs, ALU.mult, [("tmp", t_t1), ("tmp", t_rs)], out_bf_keys)
        if out_f is not None:
            o0 = max(c0, 1152)
            P.tt(out_f(o0, c1), t1[:, o0 - c0:w], rs[:, o0 - c0:w], ALU.mult, [("tmp", t_t1), ("tmp", t_rs)], out_f_keys)

    def layer(l):
        F0, F1 = FR[l]
        K0, K1 = KVR[l]
        fblocks = blocks(F0, F1)
        if l == 0:
            kvblocks = [(0, 128)] + blocks(128, TK)
        else:
            kvblocks = blocks(K0, K1)
        Wl = w_in[l]

        load_vec16(gam[:, 0, :], norm_mix[l], ("gam", l, 0))
        load_vec16(gam[:, 1, :], norm_ffn[l], ("gam", l, 1))
        load_vec16(gam[:, 2, :], norm_ple[l], ("gam", l, 2))
        P.dma(SP, pscale[:, :], pool_scale[l].rearrange("(kc p) -> p kc", p=128), s_lay, (), [("pscale",)])
        for hh in range(2):
            P.dma(SP, qkg[hh * 64:(hh + 1) * 64, 0:1], q_norm[l].rearrange("(p o) -> p o", o=1), s_lay, (), [("qkg",)])
            P.dma(SP, qkg[hh * 64:(hh + 1) * 64, 1:2], k_norm[l].rearrange("(p o) -> p o", o=1), s_lay, (), [("qkg",)])
        P.dma(SP, sk16[:, :], sinks[l:l + 1, :].to_broadcast([128, 16]), s_lay, (), [("sk16",)])
        P.group_commit(s_lay, [("gam", l, 0), ("gam", l, 1), ("gam", l, 2), ("pscale",), ("qkg",), ("sk16",)])
        for hh in range(2):
            P.act(esink[hh * 64:(hh + 1) * 64, :], sk16[hh * 64:(hh + 1) * 64, :].rearrange("p (j two) -> p j two", two=2)[:, :, hh],
                  AF.Exp, [("sk16",)], [("esink",)])
        for which in range(2):
            P.ts(Rg[:, which, :], Rm[:, :], qkg[:, which:which + 1], ALU.mult, [("Rm",), ("qkg",)], [("Rg",)])

        if l == 0:
            for rb in range(11):
                for half in range(2):
                    s = half
                    P.dma(SP, bigst[:, s, :], xin[rb * 128:(rb + 1) * 128, half * 1024:(half + 1) * 1024], s_big[s],
                          (), [("bigst", s)])
                    for jj in range(2):
                        b = bank()
                        P.job([("ps", b)])
                        for kk in range(4):
                            P.mm(psb[b][:, kk * 128:(kk + 1) * 128], bigst[:, s, (jj * 4 + kk) * 128:(jj * 4 + kk + 1) * 128],
                                 ident[:, :], True, True, [("bigst", s), ("ident",)], last=(kk == 3))
                        k0 = half * 8 + jj * 4
                        src = psb[b][:, :].rearrange("p (a c) -> p a c", a=4)
                        if rb == 0:
                            dst = tmp[:, 0:4, :].rearrange("p a b -> p (a b)").rearrange("p (k c) -> p k c", k=NCH)[:, k0:k0 + 4, :]
                            wk = [("tmp", t) for t in range(4)]
                        else:
                            dst = xv[:, k0:k0 + 4, (rb - 1) * 128:rb * 128]
                            wk = [("x", k0 + i, rb) for i in range(4)]
                        P.copy(P.alt(), dst, src, [("ps", b)], wk)
            P.tmp_i = 4

        rmsnorm(l, 0, None, kvblocks)
        dbg("h%d" % l, Hb[:, :], [128, NCH * TK], BF16, [("h", k, cb) for k in range(NCH) for cb in range(11)])
        for k in range(NCH):
            P.dma(SP, xs[:, k * XW:(k + 1) * XW], xv[:, k, :], s_spill, cks("x", k, 128, TK), [("xs", k)])
        P.barrier()
        if stop_after == "h":
            return False

        P.dma(SP, cosT, c_cos, s_lay, (), [("cos",)])
        P.dma(SP, sinT, c_sin, s_lay, (), [("sin",)])
        P.group_commit(s_lay, [("cos",), ("sin",)])
        P.memset(DVE, Vze[:, :, :, :], 0.0, [("Vze", cb) for cb in range(11)])
        P.memset(DVE, Vzo[:, :, :, :], 0.0, [("Vzo", cb) for cb in range(11)])
        P.memset(DVE, Vzc[:, :, :, :], 0.0, [("Vzc",)])
        for s in range(2):
            st = bigst[:, s, 0:512].rearrange("p (g t d) -> p g t d", g=4, t=2)
            for t in range(2):
                P.dma(SP, st[:, :, t, :], ck_in[l, s].rearrange("k (g d) -> k g d", g=4), s_big[s], (), [("bigst", s)])
            b = bank()
            P.job([("ps", b)])
            for g in range(4):
                P.mm(psb[b][:, g * 128:(g + 1) * 128], bigst[:, s, g * 128:(g + 1) * 128], ident[:, :], True, True,
                     [("bigst", s), ("ident",)], last=(g == 3))
            P.copy(DVE, kTc[:, :, s, :], psb[b][:, :].rearrange("p (g k) -> p g k", g=4), [("ps", b)], [("kTc", s)])
            P.dma(SP, bigst[:, s, 0:256], cv_in[l, s], s_big[s], (), [("bigst", s)])
            P.copy(DVE, Vzc[:, s, :, 64:128], bigst[:, s, 0:256].rearrange("p (g d) -> p g d", g=4),
                   [("bigst", s)], [("Vzc",)])
            P.dma(SP, nks[l, s, 0:64, :], ck_in[l, s, 64:128, :], s_out, (), ())
            P.dma(SP, nvs[l, s, 0:64, :], cv_in[l, s, 64:128, :], s_out, (), ())

        if stop_after == "kvpre":
            return False
        for t in range(2):
            def dstf(gg_, dupi):
                return lambda s_: s_.rearrange("p (kc gg u d) -> p kc gg u d", kc=16, gg=2, u=2)[:, :, gg_, dupi, :]

            def srcf(gg_):
                c_ = C_K + 128 * t + 64 * gg_
                return Wl[:, c_:c_ + 64].rearrange("(kc p) d -> p kc d", p=128)
            slot, wkey = wload([(dstf(gg_, du), srcf(gg_)) for gg_ in range(2) for du in range(2)])
            wt = slot.rearrange("p (kc n) -> p kc n", kc=16)
            for gg in range(2):
                g = 2 * t + gg
                for (c0, c1) in blocks(K0, K1):
                    w = c1 - c0
                    b = bank()
                    P.job([("ps", b)])
                    for k in range(NCH):
                        P.mm(psb[b][:, 0:w], wt[:, k, gg * 128:(gg + 1) * 128], hv[:, k, c0:c1], k == 0, k == NCH - 1,
                             [wkey] + cks("h", k, c0, c1), last=(k == NCH - 1))
                    outf = (lambda o0, o1, g=g: kf[:, g, o0 - 1152:o1 - 1152]) if c1 > 1152 else None
                    qk_chain(b, w, c0, c1, 1, kT[:, g, c0:c1], cks("kT", g, c0, c1), outf, [("kf", g)])
        if stop_after == "k":
            return False
        wv, wvkey = w_k16(Wl, C_V)
        for grid in range(2):
            for cb in range(11):
                t0 = cb * 128 + 64 * grid
                t1 = min(t0 + 128, TK)
                if t0 < K0 or t0 >= TK:
                    continue
                nt = t1 - t0
                b = bank()
                P.job([("ps", b)])
                for k in range(NCH):
                    P.mm(psb[b][0:nt, 0:256], hv[:, k, t0:t1], wv[:, k, :], k == 0, k == NCH - 1,
                         [wvkey] + cks("h", k, t0, t1), last=(k == NCH - 1))
                Vz = Vze if grid == 0 else Vzo
                P.copy(DVE, Vz[0:nt, cb, :, 64:128], psb[b][0:nt, 0:256].rearrange("p (g d) -> p g d", g=4),
                       [("ps", b)], [("Vze" if grid == 0 else "Vzo", cb)])
                if grid == 0 and cb in (9, 10):
                    ts_ = tslot()
                    P.copy(DVE, tmp[:, ts_, 0:256], psb[b][:, 0:256], [("ps", b)], [("tmp", ts_)])
                    if cb == 9:
                        P.dma(SP, nvp[l], tmp[:, ts_, 0:256], s_tst[ts_], [("tmp", ts_)], ())
                    else:
                        for s in range(2):
                            P.dma(SP, nvs[l, s, 64:128, :], tmp[64 * s:64 * s + 64, ts_, 0:256], s_tst[ts_], [("tmp", ts_)], ())
        if stop_after == "v":
            return False
        for part in range(2):
            b = bank()
            P.job([("ps", b)])
            for g in range(4):
                P.mm(psb[b][:, g * 64:(g + 1) * 64], kf[0:64, g, part * 128:(part + 1) * 128], ident[0:64, 0:64], True, True,
                     [("kf", g), ("ident",)], last=(g == 3))
            ts_ = tslot()
            P.copy(DVE, tmp[:, ts_, 0:256], psb[b][:, 0:256], [("ps", b)], [("tmp", ts_)])
            if part == 0:
                P.dma(SP, nkp[l], tmp[:, ts_, 0:256], s_tst[ts_], [("tmp", ts_)], ())
            else:
                for s in range(2):
                    P.dma(SP, nks[l, s, 64:128, :], tmp[64 * s:64 * s + 64, ts_, 0:256], s_tst[ts_], [("tmp", ts_)], ())

        for t in range(4):
            wt, wkey = w_k16(Wl, C_Q + 256 * t)
            for jj in range(2):
                j = 2 * t + jj
                for (c0, c1) in fblocks:
                    w = c1 - c0
                    b = bank()
                    P.job([("ps", b)])
                    for k in range(NCH):
                        P.mm(psb[b][:, 0:w], wt[:, k, jj * 128:(jj + 1) * 128], hv[:, k, c0:c1], k == 0, k == NCH - 1,
                             [wkey] + cks("h", k, c0, c1), last=(k == NCH - 1))
                    qk_chain(b, w, c0, c1, 0, qoap(j, c0, c1), cks("qo", j, c0, c1))
        dbg("q%d" % l, arena[:, 10240:15360], [128, 5120], F32, [("qo", j, cb) for j in range(8) for cb in range(11)])
        dbg("kT%d" % l, arena[:, 0:2816], [128, 2816], F32, [("kT", g, cb) for g in range(4) for cb in range(11)])
        if stop_after == "q":
            return False

        units = []
        for c in range(F0 // 64, 20):
            if c % 2 == 0:
                VA, VB = (Vze, (c - 2) // 2, "Vze"), (Vze, c // 2, "Vze")
            else:
                VA, VB = (Vzo, (c - 3) // 2, "Vzo"), (Vzo, (c - 1) // 2, "Vzo")
            units.append(dict(q0=64 * c, kA=lambda g, c=c: kT[:, g, 64 * (c - 2):64 * c], kAk=lambda g, c=c: cks("kT", g, 64 * (c - 2), 64 * c),
                              VA=VA, bA=c - 2, kB0=64 * c, VB=VB, bB=c))
        for s in range(2):
            VB = (Vze, 10, "Vze") if s == 0 else (Vzo, 10, "Vzo")
            units.append(dict(q0=C_S[s], kA=lambda g, s=s: kTc[:, g, s, :], kAk=lambda g, s=s: [("kTc", s)],
                              VA=(None, s, "Vzc"), bA=23, kB0=C_S[s], VB=VB, bB=23))
        items = [(u, j) for u in units for j in range(8)]

        def stage_a(i):
            u, j = items[i]
            q0 = u["q0"]
            g = j // 2
            r = i % 4
            qkeys = cks("qo", j, q0, q0 + 64)
            b = bank()
            P.job([("ps", b)])
            kA = u["kA"](g)
            kB = kT[:, g, u["kB0"]:u["kB0"] + 64]
            kBk = cks("kT", g, u["kB0"], u["kB0"] + 64)
            qa = qoap(j, q0, q0 + 64)
            P.copy(DVE, qbd[0:64, r, 0:64], qa[0:64, :], qkeys, [("qbd", r)])
            P.copy(DVE, qbd[64:128, r, 64:128], qa[64:128, :], qkeys, [("qbd", r)])
            P.mm(psb[b][:, 0:128], kA, qbd[:, r, :], True, True, u["kAk"](g) + [("qbd", r)])
            P.mm(psb[b][0:64, 128:256], kB, qbd[:, r, :], True, True, kBk, last=True)
            P.act(pTa[:, r, :], psb[b][:, 0:128], AF.Exp, [("ps", b), ("kbias",)], [("pTa", r)],
                  bias=kbias[:, u["bA"]:u["bA"] + 1], scale=0.125)
            P.act(pTb[0:64, r, :], psb[b][0:64, 128:256], AF.Exp, [("ps", b), ("kbias",)], [("pTb", r)],
                  bias=kbias[0:64, u["bB"]:u["bB"] + 1], scale=0.125)

        def stage_b(i):
            u, j = items[i]
            q0 = u["q0"]
            g = j // 2
            r = i % 4
            qkeys = cks("qo", j, q0, q0 + 64)
            qa = qoap(j, q0, q0 + 64)
            VzA, ia, na = u["VA"]
            VzB, ib, nb = u["VB"]
            if VzA is None:
                vA = Vzc[:, ia, g, :]
                vAk = [("Vzc",)]
            else:
                vA = VzA[:, ia, g, :]
                vAk = [(na, ia)]
            vB = VzB[:, ib, g, :]
            vBk = [(nb, ib)]
            b2 = bank()
            P.job([("ps", b2)])
            P.mm(psb[b2][:, 0:64], vA[:, 64:192], pTa[:, r, 0:64], True, False, vAk + [("pTa", r)])
            P.mm(psb[b2][:, 0:64], vA[:, 0:128], pTa[:, r, 64:128], False, False)
            P.mm(psb[b2][:, 0:64], vB[:, 64:192], pTb[:, r, 0:64], False, False, vBk + [("pTb", r)])
            P.mm(psb[b2][:, 0:64], vB[:, 0:128], pTb[:, r, 64:128], False, True)
            P.mm(psb[b2][:, 64:128], onesz[:, 64:192], pTa[:, r, 0:64], True, False, [("onesz",)])
            P.mm(psb[b2][:, 64:128], onesz[:, 0:128], pTa[:, r, 64:128], False, False)
            P.mm(psb[b2][:, 64:128], onesz[:, 64:192], pTb[:, r, 0:64], False, False)
            P.mm(psb[b2][:, 64:128], onesz[:, 0:128], pTb[:, r, 64:128], False, True, last=True)
            dn = dent[:, r, :]
            P.ts(dn, psb[b2][:, 64:128], esink[:, j:j + 1], ALU.add, [("ps", b2), ("esink",)], [("dent", r)])
            P.op(DVE, lambda e, dn=dn: e.reciprocal(out=dn, in_=dn), [("dent", r)], [("dent", r)])
            P.tt(qa, psb[b2][:, 0:64], dn, ALU.mult, [("ps", b2), ("dent", r)], qkeys)

        stage_a(0)
        for i in range(1, len(items)):
            stage_a(i)
            stage_b(i - 1)
        stage_b(len(items) - 1)
        dbg("o%d" % l, arena[:, 10240:15360], [128, 5120], F32, [("qo", j, cb) for j in range(8) for cb in range(11)])
        P.barrier()
        if stop_after == "attn":
            return False

        for s in range(2):
            P.dma(SP, bigst[0:15, s, :], sp_in[l, s], s_big[s], (), [("bigst", s)])
            b = bank()
            P.job([("ps", b)])
            for uc in range(8):
                P.mm(psb[b][:, uc * 16 + 1:uc * 16 + 16], bigst[0:15, s, uc * 128:(uc + 1) * 128], ident[0:15, 0:15], True, True,
                     [("bigst", s), ("ident",)], last=(uc == 7))
            P.copy(DVE, histT[:, :, s, 1:16], psb[b][:, 0:128].rearrange("p (u c) -> p u c", u=8)[:, :, 1:16],
                   [("ps", b)], [("histT", s)])
        ub, sA, sB = ub3[:, 0, :], ub3[:, 1, :], ub3[:, 2, :]
        UW = 1440

        def umap(c0, c1):
            out = []
            segs = ((0, 1280, 0), (1280, 1344, 1296), (1344, 1408, 1376))
            for (a0, a1, o) in segs:
                lo, hi = max(c0, a0), min(c1, a1)
                if lo < hi:
                    out.append((lo - c0, hi - lo, o + lo - a0))
            return out

        for t in range(4):
            wt, wkey = w_k16(Wl, C_U + 256 * t)
            for jj in range(2):
                uc = 2 * t + jj
                gi = uc // 2
                for (c0, c1) in blocks(K0, K1):
                    w = c1 - c0
                    b = bank()
                    P.job([("ps", b)])
                    for k in range(NCH):
                        P.mm(psb[b][:, 0:w], wt[:, k, jj * 128:(jj + 1) * 128], hv[:, k, c0:c1], k == 0, k == NCH - 1,
                             [wkey] + cks("h", k, c0, c1), last=(k == NCH - 1))
                    for (so, n, uo) in umap(c0, c1):
                        P.copy(ACT, ub[:, uo:uo + n], psb[b][:, so:so + n], [("ps", b)], [("ub",)])
                for s in range(2):
                    P.copy(DVE, ub[:, 1280 + 80 * s:1296 + 80 * s], histT[:, uc, s, :], [("histT", s)], [("ub",)])
                if l == 1:
                    P.memset(DVE, ub[:, 0:128], 0.0, [("ub",)])
                P.memset(DVE, ub[:, 1280:1281], 0.0, [("ub",)])
                P.memset(DVE, ub[:, 1360:1361], 0.0, [("ub",)])
                P.tt(sA[:, 1:UW], ub[:, 1:UW], ub[:, 0:UW - 1], ALU.add, [("ub",)], [("sA",)])
                fin, fk = sA, ("sA",)
                if gi >= 1:
                    P.tt(sB[:, 3:UW], sA[:, 3:UW], sA[:, 1:UW - 2], ALU.add, [("sA",)], [("sB",)])
                    fin, fk = sB, ("sB",)
                if gi >= 2:
                    P.tt(sA[:, 7:UW], sB[:, 7:UW], sB[:, 3:UW - 4], ALU.add, [("sB",)], [("sA",)])
                    fin, fk = sA, ("sA",)
                if gi >= 3:
                    P.tt(sB[:, 15:UW], sA[:, 15:UW], sA[:, 7:UW - 8], ALU.add, [("sA",)], [("sB",)])
                    fin, fk = sB, ("sB",)
                wnd = 2 ** (gi + 1)
                for (so, n, uo) in umap(F0, F1):
                    cc0 = F0 + so
                    P.stt(dpap(uc, cc0, cc0 + n), fin[:, uo:uo + n], 1.0 / wnd, ub[:, uo:uo + n], ALU.mult, ALU.subtract,
                          [fk, ("ub",)], cks("dp", uc, cc0, cc0 + n))
                ts_ = tslot()
                tf = tmp[:, ts_, 0:16]
                P.tt(tf, fin[:, C_MAIN:C_MAIN + 16], invcnt[:, uc, :], ALU.mult, [fk, ("invcnt",)], [("tmp", ts_)])
                P.tt(dpap(uc, C_MAIN, C_MAIN + 16), tf, ub[:, C_MAIN:C_MAIN + 16], ALU.subtract, [("tmp", ts_), ("ub",)],
                     cks("dp", uc, C_MAIN, C_MAIN + 16))
                b = bank()
                P.job([("ps", b)])
                offs = (1265, 1296 + 49, 1376 + 49)
                for i, o in enumerate(offs):
                    P.mm(psb[b][0:15, i * 128:(i + 1) * 128], ub[:, o:o + 15], ident[:, :], True, True, [("ub",), ("ident",)],
                         last=(i == 2))
                ps_ = 0
                P.copy(DVE, pst[0:15, ps_, :, :], psb[b][0:15, 0:384].rearrange("p (a c) -> p a c", a=3), [("ps", b)], [("pst", ps_)])
                P.dma(SP, npp[l][:, uc * 128:(uc + 1) * 128], pst[0:15, ps_, 0, :], s_pst[ps_], [("pst", ps_)], ())
                for s in range(2):
                    P.dma(SP, nps[l, s][:, uc * 128:(uc + 1) * 128], pst[0:15, ps_, 1 + s, :], s_pst[ps_], [("pst", ps_)], ())
        dbg("d%d" % l, arena[:, 15360:20480], [128, 5120], F32, [("dp", j, cb) for j in range(8) for cb in range(11)])

        slot, wkey = wload([(lambda s_: s_[:, 0:2048].rearrange("p (a b) -> p a b", a=8),
                             pool_w[l].rearrange("g (cc p) d -> p (g cc) d", p=128))])
        wt = slot[:, 0:2048].rearrange("p (a b) -> p a b", a=8)
        for gi in range(4):
            for (c0, c1) in fblocks:
                w = c1 - c0
                bs = []
                for dh in range(2):
                    b = bank()
                    bs.append(b)
                    P.job([("ps", b)])
                    for cc in range(2):
                        P.mm(psb[b][:, 0:w], wt[:, 2 * gi + cc, dh * 128:(dh + 1) * 128], dpap(2 * gi + cc, c0, c1), cc == 0, cc == 1,
                             [wkey] + cks("dp", 2 * gi + cc, c0, c1), last=(cc == 1))
                for dh in range(2):
                    P.ts(dpap(2 * gi + dh, c0, c1), psb[bs[dh]][:, 0:w], pscale[:, 2 * gi + dh:2 * gi + dh + 1], ALU.mult,
                         [("ps", bs[dh]), ("pscale",)], cks("dp", 2 * gi + dh, c0, c1))
        dbg("pl%d" % l, arena[:, 15360:20480], [128, 5120], F32, [("dp", j, cb) for j in range(8) for cb in range(11)])
        P.barrier()
        if stop_after == "pool":
            return False

        for pr in range(8):
            def half(i):
                return lambda s_: s_[:, 2048 * i:2048 * (i + 1)].rearrange("p (a b) -> p a b", a=8)
            slot, wbk = wload([(half(0), w_attn_br[l][:, pr * 256:(pr + 1) * 256].rearrange("(kc p) n -> p kc n", p=128)),
                               (half(1), w_pool_br[l][:, pr * 256:(pr + 1) * 256].rearrange("(kc p) n -> p kc n", p=128))])
            wab = slot[:, 0:2048].rearrange("p (a b) -> p a b", a=8)
            wpb = slot[:, 2048:4096].rearrange("p (a b) -> p a b", a=8)
            wga, wgak = w_k16(Wl, C_GA + 256 * pr)
            wgb, wgbk = w_k16(Wl, C_GB + 256 * pr)
            for jj in range(2):
                n = 2 * pr + jj
                for (c0, c1) in fblocks:
                    w = c1 - c0
                    ba, bga, bc, bgb = bank(), bank(), bank(), bank()
                    P.job([("ps", ba)])
                    for k in range(8):
                        P.mm(psb[ba][:, 0:w], wab[:, k, jj * 128:(jj + 1) * 128], qoap(k, c0, c1), k == 0, k == 7,
                             [wbk] + cks("qo", k, c0, c1), last=(k == 7))
                    P.job([("ps", bga)])
                    for k in range(NCH):
                        P.mm(psb[bga][:, 0:w], wga[:, k, jj * 128:(jj + 1) * 128], hv[:, k, c0:c1], k == 0, k == NCH - 1,
                             [wgak] + cks("h", k, c0, c1), last=(k == NCH - 1))
                    P.job([("ps", bc)])
                    for k in range(8):
                        P.mm(psb[bc][:, 0:w], wpb[:, k, jj * 128:(jj + 1) * 128], dpap(k, c0, c1), k == 0, k == 7,
                             [wbk] + cks("dp", k, c0, c1), last=(k == 7))
                    P.job([("ps", bgb)])
                    for k in range(NCH):
                        P.mm(psb[bgb][:, 0:w], wgb[:, k, jj * 128:(jj + 1) * 128], hv[:, k, c0:c1], k == 0, k == NCH - 1,
                             [wgbk] + cks("h", k, c0, c1), last=(k == NCH - 1))
                    t1_, t2_ = tslot(), tslot()
                    s1, s2 = tmp[:, t1_, 0:w], tmp[:, t2_, 0:w]
                    P.act(s1, psb[bga][:, 0:w], AF.Sigmoid, [("ps", bga)], [("tmp", t1_)])
                    P.act(s2, psb[bgb][:, 0:w], AF.Sigmoid, [("ps", bgb)], [("tmp", t2_)])
                    P.tt(s1, s1, psb[ba][:, 0:w], ALU.mult, [("tmp", t1_), ("ps", ba)], [("tmp", t1_)])
                    P.tt(s2, s2, psb[bc][:, 0:w], ALU.mult, [("tmp", t2_), ("ps", bc)], [("tmp", t2_)])
                    P.tt(map_(n, c0, c1), s1, s2, ALU.add, [("tmp", t1_), ("tmp", t2_)], cks("m", n, c0, c1))
        P.barrier()

        xr_i = 0
        for pr in range(8):
            wt, wkey = w_k16(w_out[l], 256 * pr)
            for jj in range(2):
                n = 2 * pr + jj
                for (c0, c1) in fblocks:
                    w = c1 - c0
                    b = bank()
                    P.job([("ps", b)])
                    for k in range(NCH):
                        P.mm(psb[b][:, 0:w], wt[:, k, jj * 128:(jj + 1) * 128], map_(k, c0, c1), k == 0, k == NCH - 1,
                             [wkey] + cks("m", k, c0, c1), last=(k == NCH - 1))
                    ts_ = xr_i % 8
                    xr_i += 1
                    P.dma(SP, tmp[:, ts_, 0:w], xs[:, n * XW + c0 - 128:n * XW + c1 - 128], s_xr[ts_], [("xs", n)], [("tmp", ts_)])
                    if n >= 8:
                        dst, dk = xv[:, n, c0 - 128:c1 - 128], cks("x", n, c0, c1)
                    else:
                        dst, dk = xlo[:, n, c0 - 128:c1 - 128], cks("xlo", n, c0, c1)
                    P.tt(dst, tmp[:, ts_, 0:w], psb[b][:, 0:w], ALU.add, [("tmp", ts_), ("ps", b)], dk)
        P.barrier()
        for n in range(8):
            for (c0, c1) in fblocks:
                P.copy(P.alt(), xv[:, n, c0 - 128:c1 - 128], xlo[:, n, c0 - 128:c1 - 128], cks("xlo", n, c0, c1), cks("x", n, c0, c1))
        P.barrier()
        dbg("x1_%d" % l, arena[:, :], [128, NCH * XW], F32, [("x", k, cb) for k in range(NCH) for cb in range(1, 11)])
        if stop_after == "mix":
            return False

        P.tmp_i = 0
        if l % 2 == 0:
            rmsnorm(l, 1, None, fblocks)
        else:
            P.dma(SP, rtr[:, :, :], moe_router[0].rearrange("(kc p) e -> p kc e", p=128), s_lay, (), [("rtr",)])
            state = {"n": 0}
            ntb = (F1 - F0) // 128

            def router_block(c0, c1, rs, rsk):
                for i in range((c1 - c0) // 128):
                    tb_ = (c0 - F0) // 128 + i
                    a0 = c0 + i * 128
                    lb = bank()
                    P.job([("ps", lb)])
                    for k in range(NCH):
                        ts2 = 6 + (state["n"] % 2)
                        state["n"] += 1
                        hf = tmp[:, ts2, 0:128]
                        P.stt(hf, xap(k, a0, a0 + 128), gam[:, 1, k:k + 1], rs[:, i * 128:(i + 1) * 128], ALU.mult, ALU.mult,
                              xck(k, a0, a0 + 128) + [rsk, ("gam", l, 1)], [("tmp", ts2)])
                        P.mm(psb[lb][:, 0:8], hf, rtr[:, k, :], k == 0, k == NCH - 1, [("tmp", ts2), ("rtr",)],
                             last=(k == NCH - 1), inc=True)
                    P.copy(DVE, lg[:, tb_, :], psb[lb][:, 0:8], [("ps", lb)], [("lg", tb_)])

            P.tmp_i = 0
            rmsnorm(l, 1, None, fblocks, want_f32=router_block)
            P.tmp_i = 0
            for tb_ in range(ntb):
                L = lg[:, tb_, :]
                sc = rsm[:, tb_, :]
                P.op(DVE, lambda e, L=L, sc=sc: e.tensor_reduce(out=sc[:, 0:1], in_=L, axis=AX.X, op=ALU.max), [("lg", tb_)], [("rsm", tb_)])
                P.ts(sc[:, 2:3], sc[:, 0:1], -1.0, ALU.mult, [("rsm", tb_)], [("rsm", tb_)])
                c_ = comb[:, tb_, :]
                P.ts(c_, L, sc[:, 0:1], ALU.is_equal, [("lg", tb_), ("rsm", tb_)], [("comb", tb_)], s2=-1e30, op1=ALU.mult)
                P.tt(c_, c_, L, ALU.add, [("comb", tb_), ("lg", tb_)], [("comb", tb_)])
                P.op(DVE, lambda e, c_=c_, sc=sc: e.tensor_reduce(out=sc[:, 1:2], in_=c_, axis=AX.X, op=ALU.max), [("comb", tb_)], [("rsm", tb_)])
                P.ts(c_, L, sc[:, 1:2], ALU.is_ge, [("lg", tb_), ("rsm", tb_)], [("comb", tb_)])
                ex = rsm[:, tb_, 3:4]
                P.act(lg[:, tb_, :], L, AF.Exp, [("lg", tb_), ("rsm", tb_)], [("lg", tb_)], bias=sc[:, 2:3], scale=1.0)
                P.tt(c_, c_, lg[:, tb_, :], ALU.mult, [("comb", tb_), ("lg", tb_)], [("comb", tb_)])
                P.op(DVE, lambda e, c_=c_, ex=ex: e.tensor_reduce(out=ex, in_=c_, axis=AX.X, op=ALU.add), [("comb", tb_)], [("rsm", tb_)])
                P.op(DVE, lambda e, ex=ex: e.reciprocal(out=ex, in_=ex), [("rsm", tb_)], [("rsm", tb_)])
                P.ts(c_, c_, ex, ALU.mult, [("comb", tb_), ("rsm", tb_)], [("comb", tb_)])
        dbg("h2_%d" % l, Hb[:, :], [128, NCH * TK], BF16, [("h", k, cb) for k in range(NCH) for cb in range(11)])
        if l % 2 == 1:
            dbg("comb", comb[:, :, :], [128, 9, 8], F32, [("comb", t) for t in range(9)])
        if stop_after == "h2":
            return False

        cmbt = tmp[:, 5:8, :].rearrange("p a b -> p (a b)")

        def cmbgen(e):
            ntb = (F1 - F0) // 128
            for q4 in range(0, ntb, 4):
                nb_ = min(4, ntb - q4)
                b = bank()
                P.job([("ps", b)])
                for i in range(nb_):
                    tb_ = q4 + i
                    dg = tmp[:, i % 2, 0:128]
                    P.ts(dg, ident[:, :], comb[:, tb_, e:e + 1], ALU.mult, [("ident",), ("comb", tb_)], [("tmp", i % 2)])
                    P.mm(psb[b][:, i * 128:(i + 1) * 128], ones_f[:, :], dg, True, True, [("ones_f",), ("tmp", i % 2)],
                         last=(i == nb_ - 1), inc=True)
                P.copy(ACT, cmbt[:, q4 * 128:(q4 + nb_) * 128], psb[b][:, 0:nb_ * 128], [("ps", b)], [("cmb",)])

        def ffn_all(specs):
            seq = [(Wgu, Wd, e, grp) for (Wgu, Wd, e) in specs for grp in range(NHID // 2)]

            def gu(i):
                Wgu, Wd, e, grp = seq[i]
                par = i % 2
                j0 = 2 * grp
                if grp == 0 and e is not None:
                    cmbgen(e)
                wg, wgk = w_k16(Wgu, j0 * 128)
                wu, wuk = w_k16(Wgu, DFF + j0 * 128)
                for jj in range(2):
                    for (c0, c1) in fblocks:
                        w = c1 - c0
                        bg, bu = bank(), bank()
                        P.job([("ps", bg)])
                        for k in range(NCH):
                            P.mm(psb[bg][:, 0:w], wg[:, k, jj * 128:(jj + 1) * 128], hv[:, k, c0:c1], k == 0, k == NCH - 1,
                                 [wgk] + cks("h", k, c0, c1), last=(k == NCH - 1))
                        P.job([("ps", bu)])
                        for k in range(NCH):
                            P.mm(psb[bu][:, 0:w], wu[:, k, jj * 128:(jj + 1) * 128], hv[:, k, c0:c1], k == 0, k == NCH - 1,
                                 [wuk] + cks("h", k, c0, c1), last=(k == NCH - 1))
                        ts_ = 2 + (P.tmp_i % 3)
                        P.tmp_i += 1
                        sg = tmp[:, ts_, 0:w]
                        P.act(sg, psb[bg][:, 0:w], AF.Silu, [("ps", bg)], [("tmp", ts_)])
                        if e is not None:
                            P.tt(sg, sg, cmbt[:, c0 - F0:c1 - F0], ALU.mult, [("tmp", ts_), ("cmb",)], [("tmp", ts_)])
                        P.tt(actbuf[:, par, jj, c0 - 128:c1 - 128], sg, psb[bu][:, 0:w], ALU.mult, [("tmp", ts_), ("ps", bu)],
                             cks("act%d" % par, jj, c0, c1))

            def down(i):
                Wgu, Wd, e, grp = seq[i]
                par = i % 2
                j0 = 2 * grp
                slot, wdk = wload([(lambda s_: s_.rearrange("p (a b) -> p a b", a=2),
                                    Wd[j0 * 128:(j0 + 2) * 128, :].rearrange("(jj p) n -> p jj n", p=128))])
                wd = slot.rearrange("p (a b) -> p a b", a=2)
                for n in range(NCH):
                    for (c0, c1) in fblocks:
                        w = c1 - c0
                        b = bank()
                        P.job([("ps", b)])
                        for jj in range(2):
                            P.mm(psb[b][:, 0:w], wd[:, jj, n * 128:(n + 1) * 128], actbuf[:, par, jj, c0 - 128:c1 - 128], jj == 0, jj == 1,
                                 [wdk] + cks("act%d" % par, jj, c0, c1), last=(jj == 1))
                        P.tt(xv[:, n, c0 - 128:c1 - 128], xv[:, n, c0 - 128:c1 - 128], psb[b][:, 0:w], ALU.add,
                             cks("x", n, c0, c1) + [("ps", b)], cks("x", n, c0, c1))

            gu(0)
            for i in range(1, len(seq)):
                gu(i)
                down(i - 1)
            down(len(seq) - 1)

        if l % 2 == 0:
            ffn_all([(ffn_w_gate_up[l // 2], ffn_w_down[l // 2], None)])
        else:
            ffn_all([(moe_w_gate_up[l // 2, e], moe_w_down[l // 2, e], e) for e in range(NEXP)])
        dbg("x2_%d" % l, arena[:, :], [128, NCH * XW], F32, [("x", k, cb) for k in range(NCH) for cb in range(1, 11)])
        P.barrier()
        if stop_after == "ffn":
            return False

        P.tmp_i = 0
        rmsnorm(l, 2, None, fblocks)
        pT = actbuf[:, :, :, :].rearrange("p a b c -> p (a b c)")[:, 0:2 * XW].rearrange("p (k c) -> p k c", k=2)
        for rb in range((F0 - 128) // 128, 10):
            s = rb % 2
            P.dma(SP, bigst[:, s, 0:256], pin[l, rb * 128:(rb + 1) * 128, :], s_big[s], (), [("bigst", s)])
            b = bank()
            P.job([("ps", b)])
            for kc in range(2):
                P.mm(psb[b][:, kc * 128:(kc + 1) * 128], bigst[:, s, kc * 128:(kc + 1) * 128], ident[:, :], True, True,
                     [("bigst", s), ("ident",)], last=(kc == 1))
            P.copy(DVE, pT[:, :, rb * 128:(rb + 1) * 128], psb[b][:, 0:256].rearrange("p (k c) -> p k c", k=2),
                   [("ps", b)], [("pT", rb + 1)])
        for pr in range(8):
            slot, wpk = wload([(lambda s_: s_.rearrange("p (a b) -> p a b", a=2),
                                w_ple[l].rearrange("(kc p) n -> p kc n", p=128))])
            wp = slot.rearrange("p (a b) -> p a b", a=2)
            wt, wkey = w_k16(w_ple_gate[l], 256 * pr)
            for jj in range(2):
                n = 2 * pr + jj
                for (c0, c1) in fblocks:
                    w = c1 - c0
                    bg, bp = bank(), bank()
                    P.job([("ps", bg)])
                    for k in range(NCH):
                        P.mm(psb[bg][:, 0:w], wt[:, k, jj * 128:(jj + 1) * 128], hv[:, k, c0:c1], k == 0, k == NCH - 1,
                             [wkey] + cks("h", k, c0, c1), last=(k == NCH - 1))
                    P.job([("ps", bp)])
                    for kc in range(2):
                        P.mm(psb[bp][:, 0:w], wp[:, kc, n * 128:(n + 1) * 128], pT[:, kc, c0 - 128:c1 - 128], kc == 0, kc == 1,
                             [wpk] + [("pT", cb) for cb in range(c0 // 128, c1 // 128)], last=(kc == 1))
                    ts_ = tslot()
                    sg = tmp[:, ts_, 0:w]
                    P.act(sg, psb[bg][:, 0:w], AF.Sigmoid, [("ps", bg)], [("tmp", ts_)])
                    P.tt(sg, sg, psb[bp][:, 0:w], ALU.mult, [("tmp", ts_), ("ps", bp)], [("tmp", ts_)])
                    P.tt(xv[:, n, c0 - 128:c1 - 128], xv[:, n, c0 - 128:c1 - 128], sg, ALU.add,
                         cks("x", n, c0, c1) + [("tmp", ts_)], cks("x", n, c0, c1))
        dbg("x3_%d" % l, arena[:, :], [128, NCH * XW], F32, [("x", k, cb) for k in range(NCH) for cb in range(1, 11)])
        P.barrier()
        return True

    ok = True
    for l in range(nlayers):
        ok = layer(l)
        if not ok:
            break

    if ok:
        for rb in range(9):
            for half in range(2):
                s = half
                for jj in range(2):
                    b = bank()
                    P.job([("ps", b)])
                    for kk in range(4):
                        k = half * 8 + jj * 4 + kk
                        P.mm(psb[b][:, kk * 128:(kk + 1) * 128], xv[:, k, 128 + rb * 128:256 + rb * 128], ident[:, :], True, True,
                             [("x", k, rb + 2), ("ident",)], last=(kk == 3))
                    P.copy(P.alt(), bigst[:, s, jj * 512:(jj + 1) * 512], psb[b][:, :], [("ps", b)], [("bigst", s)])
                P.dma(SP, y_out[rb * 128:(rb + 1) * 128, half * 1024:(half + 1) * 1024], bigst[:, s, :], s_big[s], [("bigst", s)], ())

    for d in P.dsems:
        if d.issued > 0:
            P.need(SP, Tok(d.sem, d.issued))
    for E in (PE, ACT, DVE):
        if E.count > 0:
            P.need(SP, Tok(E.sem, E.count))

    def run_q(E):
        def f(e):
            for it in E.q:
                if it[0] == "w":
                    e.wait_ge(it[1], it[2])
                else:
                    ins = it[1](e)
                    if it[2] is not None:
                        ins.then_inc(it[2], it[3])
        return f

    with nc.Block() as block:
        block.tensor(run_q(PE))
        block.scalar(run_q(ACT))
        block.vector(run_q(DVE))
        block.gpsimd(run_q(POOL))
        block.sync(run_q(SP))
    es.close()
    stats = {E.name: (len(E.q), E.count) for E in P.engs}
    return nc, dbg_list, stats


def _consts(core):
    qd = core % 4
    s0 = qd * 1024
    pos = np.zeros(TK, np.float64)
    pos[:1280] = s0 - 256 + np.arange(1280)
    pos[1280:1344] = PAST + np.arange(64)
    pos[1344:1408] = PAST + np.arange(64)
    inv = (ROPE_THETA ** (-np.arange(0, 16, 2, dtype=np.float32) / 16)).astype(np.float32)
    ang = pos.astype(np.float32)[None, :] * inv[:, None]
    cos = np.ones((128, TK), np.float32)
    sin = np.zeros((128, TK), np.float32)
    for hh in range(2):
        for r in range(2):
            cos[hh * 64 + r * 8:hh * 64 + r * 8 + 8] = np.cos(ang)
            sin[hh * 64 + r * 8:hh * 64 + r * 8 + 8] = np.sin(ang)
    R = np.zeros((128, 128), np.float32)
    for hh in range(2):
        for i in range(8):
            R[hh * 64 + i + 8, hh * 64 + i] = -1.0
            R[hh * 64 + i, hh * 64 + i + 8] = 1.0
    kb = np.zeros((128, 24), np.float32)
    for cs in range(20):
        p = s0 - 256 + 64 * cs + np.arange(128)
        kb[:, cs] = np.where(p < 0, NEG, 0.0)
    ic = np.zeros((128, 8, 16), np.float32)
    for uc in range(8):
        w = 2 ** (uc // 2 + 1)
        p = s0 + np.arange(16)
        ic[:, uc, :] = (1.0 / np.minimum(w, p + 1))[None, :]
    return dict(c_ident=np.eye(128, dtype=np.float32), c_R=R, c_cos=cos, c_sin=sin, c_kbias=kb, c_invcnt=ic)


def _core_inputs(core, inp):
    b, qd = core // 4, core % 4
    s0 = qd * 1024
    m = {}
    xin = np.zeros((TK, D), np.float32)
    lo = s0 - 256
    a = max(lo, 0)
    xin[a - lo:1280] = inp["x_prompt"][b, a:s0 + 1024]
    xin[1280:1344] = inp["x_sample"][2 * core]
    xin[1344:1408] = inp["x_sample"][2 * core + 1]
    m["xin"] = xin
    pin = np.zeros((DEPTH, XW, 256), np.float32)
    lo = s0 - 128
    a = max(lo, 0)
    pin[:, a - lo:1152] = inp["p_prompt"][:, b, a:s0 + 1024]
    pin[:, 1152:1216] = inp["p_sample"][:, 2 * core]
    pin[:, 1216:1280] = inp["p_sample"][:, 2 * core + 1]
    m["pin"] = pin
    m["ck"] = np.ascontiguousarray(inp["cache_k"][:, 2 * core:2 * core + 2].reshape(DEPTH, 2, 128, 256))
    m["cv"] = np.ascontiguousarray(inp["cache_v"][:, 2 * core:2 * core + 2].reshape(DEPTH, 2, 128, 256))
    m["sp"] = np.ascontiguousarray(inp["state_pool"][:, 2 * core:2 * core + 2])
    m.update(_consts(core))
    return m


WEIGHTS = ["norm_mix", "w_in", "q_norm", "k_norm", "sinks", "pool_w", "pool_scale", "w_attn_br", "w_pool_br", "w_out",
           "norm_ffn", "ffn_w_gate_up", "ffn_w_down", "moe_router", "moe_w_gate_up", "moe_w_down", "norm_ple", "w_ple",
           "w_ple_gate"]


def kernel(**inputs):
    inp = {k: np.asarray(v) for k, v in inputs.items()}
    nc, _, _ = build_program()
    wts = {k: np.ascontiguousarray(inp[k], dtype=np.float32) for k in WEIGHTS}
    in_maps = []
    for c in range(NCORES):
        m = _core_inputs(c, inp)
        m.update(wts)
        in_maps.append(m)
    res = run_bass_kernel_spmd(nc, in_maps, core_ids=list(range(NCORES)))
    r = res.results
    y_prompt = np.zeros((2, 4096, D), np.float32)
    y_sample = np.zeros((16, 64, D), np.float32)
    nkp = np.zeros((DEPTH, 2, 128, 4, 64), np.float32)
    nvp = np.zeros((DEPTH, 2, 128, 4, 64), np.float32)
    npp = np.zeros((DEPTH, 2, 15, 1024), np.float32)
    nks = np.zeros((DEPTH, 16, 128, 4, 64), np.float32)
    nvs = np.zeros((DEPTH, 16, 128, 4, 64), np.float32)
    nps = np.zeros((DEPTH, 16, 15, 1024), np.float32)
    for c in range(NCORES):
        b, qd = c // 4, c % 4
        y_prompt[b, qd * 1024:(qd + 1) * 1024] = r[c]["y"][0:1024]
        y_sample[2 * c] = r[c]["y"][1024:1088]
        y_sample[2 * c + 1] = r[c]["y"][1088:1152]
        if qd == 3:
            nkp[:, b] = r[c]["nkp"].reshape(DEPTH, 128, 4, 64)
            nvp[:, b] = r[c]["nvp"].reshape(DEPTH, 128, 4, 64)
            npp[:, b] = r[c]["npp"]
        nks[:, 2 * c:2 * c + 2] = r[c]["nks"].reshape(DEPTH, 2, 128, 4, 64)
        nvs[:, 2 * c:2 * c + 2] = r[c]["nvs"].reshape(DEPTH, 2, 128, 4, 64)
        nps[:, 2 * c:2 * c + 2] = r[c]["nps"]
    return (y_prompt, y_sample, nkp, nvp, npp, nks, nvs, nps)
```

```python
import math
from contextlib import ExitStack

import numpy as np
import ml_dtypes
import concourse.bass as bass
import concourse.mybir as mybir
from concourse.bass_utils import run_bass_kernel_spmd

F32 = mybir.dt.float32
BF16 = mybir.dt.bfloat16
ALU = mybir.AluOpType
AF = mybir.ActivationFunctionType
AX = mybir.AxisListType

NCORES = 8
D = 2048
NCH = 16
DEPTH = 2
TK = 1408
XW = 1280
C_MAIN = 256
C_S = (1280, 1344)
FR = ((128, 1408), (256, 1408))
KVR = ((0, 1408), (128, 1408))
IN_W = 6656
C_Q, C_K, C_V, C_U, C_GA, C_GB = 0, 1024, 1280, 1536, 2560, 4608
DFF = 5632
NHID = 44
NEXP = 8
EPS = 1e-6
ROPE_THETA = 500000.0
PAST = 2048
NEG = -30000.0


def blocks(c0, c1, maxw=512):
    n = -(-(c1 - c0) // maxw)
    w = -(-((c1 - c0) // 128) // n) * 128
    out = []
    c = c0
    while c < c1:
        out.append((c, min(c + w, c1)))
        c += w
    return out


class Tok:
    __slots__ = ("sem", "val")

    def __init__(self, sem, val):
        self.sem = sem
        self.val = val


class Cell:
    __slots__ = ("w", "r")

    def __init__(self):
        self.w = None
        self.r = {}


class Eng:
    def __init__(self, name, sem):
        self.name = name
        self.sem = sem
        self.count = 0
        self.q = []
        self.waited = {}


class DSem:
    def __init__(self, sem):
        self.sem = sem
        self.issued = 0


class Prog:
    def __init__(self, nc, es, debug=None):
        self.nc = nc
        self.es = es
        self.cells = {}
        self.debug = debug or {}
        self.dbg_outs = {}
        mk = lambda n: es.enter_context(nc.semaphore(n))
        self.PE = Eng("pe", mk("s_pe"))
        self.ACT = Eng("act", mk("s_act"))
        self.DVE = Eng("dve", mk("s_dve"))
        self.POOL = Eng("pool", mk("s_pool"))
        self.SP = Eng("sp", mk("s_sp"))
        self.engs = [self.PE, self.ACT, self.DVE, self.POOL, self.SP]
        self.dsems = []
        self.bank_i = 0
        self.tmp_i = 0
        self.rr = 0

    def dsem(self, name):
        d = DSem(self.es.enter_context(self.nc.semaphore(name)))
        self.dsems.append(d)
        return d

    def cell(self, key):
        c = self.cells.get(key)
        if c is None:
            c = Cell()
            self.cells[key] = c
        return c

    def need(self, E, tok):
        if tok is None:
            return
        if E is self.PE and tok.sem is self.PE.sem:
            return
        k = id(tok.sem)
        if E.waited.get(k, 0) >= tok.val:
            return
        E.waited[k] = tok.val
        E.q.append(("w", tok.sem, tok.val))

    def _deps(self, E, reads, writes):
        for key in reads:
            self.need(E, self.cell(key).w)
        for key in writes:
            c = self.cell(key)
            self.need(E, c.w)
            for s, v in c.r.values():
                self.need(E, Tok(s, v))

    def _commit(self, tok, reads, writes):
        k = id(tok.sem)
        for key in reads:
            c = self.cell(key)
            old = c.r.get(k)
            if old is None or old[1] < tok.val:
                c.r[k] = (tok.sem, tok.val)
        for key in writes:
            c = self.cell(key)
            c.w = tok
            c.r = {}

    def op(self, E, fn, reads=(), writes=()):
        self._deps(E, reads, writes)
        E.count += 1
        tok = Tok(E.sem, E.count)
        E.q.append(("i", fn, E.sem, 1))
        self._commit(tok, reads, writes)
        return tok

    def dma(self, E, out, in_, ds, reads=(), writes=(), commit_reads=True):
        self._deps(E, reads, writes)
        ds.issued += 16
        tok = Tok(ds.sem, ds.issued)
        E.q.append(("i", lambda e: e.dma_start(out=out, in_=in_), ds.sem, 16))
        self._commit(tok, reads if commit_reads else (), writes)
        return tok

    def job(self, writes):
        self._deps(self.PE, (), writes)
        self.jwrites = list(writes)

    def mm(self, out, lhsT, rhs, start, stop, reads=(), last=False, inc=False):
        PE = self.PE
        for key in reads:
            self.need(PE, self.cell(key).w)
        tok = Tok(PE.sem, PE.count + 1)
        self._commit(tok, reads, ())
        if last or inc:
            PE.count += 1
            PE.q.append(("i", lambda e: e.matmul(out, lhsT=lhsT, rhs=rhs, start=start, stop=stop), PE.sem, 1))
            if last:
                self._commit(tok, (), self.jwrites)
        else:
            PE.q.append(("i", lambda e: e.matmul(out, lhsT=lhsT, rhs=rhs, start=start, stop=stop), None, 0))

    def group_commit(self, ds, keys):
        tok = Tok(ds.sem, ds.issued)
        for key in keys:
            self.cell(key).w = tok

    def barrier(self, engs=None):
        engs = engs or [self.PE, self.ACT, self.DVE, self.SP]
        for E in engs:
            for F in [self.PE, self.ACT, self.DVE]:
                if F.count > 0:
                    self.need(E, Tok(F.sem, F.count))
            for d in self.dsems:
                if d.issued > 0 and not getattr(d, "ring", False):
                    self.need(E, Tok(d.sem, d.issued))

    def act(self, out, in_, func, reads, writes, bias=None, scale=None):
        kw = {}
        if bias is not None:
            kw["bias"] = bias
        if scale is not None:
            kw["scale"] = scale
        return self.op(self.ACT, lambda e: e.activation(out=out, in_=in_, func=func, **kw), reads, writes)

    def tt(self, out, in0, in1, op, reads, writes, E=None):
        return self.op(E or self.DVE, lambda e: e.tensor_tensor(out=out, in0=in0, in1=in1, op=op), reads, writes)

    def ts(self, out, in0, s1, op0, reads, writes, s2=None, op1=None, E=None):
        if op1 is None:
            return self.op(E or self.DVE, lambda e: e.tensor_scalar(out=out, in0=in0, scalar1=s1, scalar2=None, op0=op0),
                           reads, writes)
        return self.op(E or self.DVE,
                       lambda e: e.tensor_scalar(out=out, in0=in0, scalar1=s1, scalar2=s2, op0=op0, op1=op1), reads, writes)

    def stt(self, out, in0, scalar, in1, op0, op1, reads, writes, E=None):
        return self.op(E or self.DVE,
                       lambda e: e.scalar_tensor_tensor(out=out, in0=in0, scalar=scalar, in1=in1, op0=op0, op1=op1),
                       reads, writes)

    def copy(self, E, out, in_, reads, writes):
        if E is self.ACT:
            return self.op(E, lambda e: e.copy(out=out, in_=in_), reads, writes)
        return self.op(E, lambda e: e.tensor_copy(out=out, in_=in_), reads, writes)

    def memset(self, E, ap, val, writes):
        return self.op(E, lambda e: e.memset(ap, val), (), writes)

    def alt(self):
        self.rr ^= 1
        return self.ACT if self.rr else self.DVE


def cks(name, chunk, c0, c1):
    return [(name, chunk, cb) for cb in range(c0 // 128, -(-c1 // 128))]


def build_program(debug=None, stop_after=None, nlayers=DEPTH, tiny_moe=False):
    debug = debug or {}
    nc = bass.Bass("TRN2", target_bir_lowering=False)
    es = ExitStack()

    def din(name, shape, dt=F32):
        return nc.dram_tensor(name, list(shape), dt, kind="ExternalInput").ap()

    def dout(name, shape, dt=F32):
        return nc.dram_tensor(name, list(shape), dt, kind="ExternalOutput").ap()

    xin = din("xin", [TK, D])
    pin = din("pin", [DEPTH, XW, 256])
    ck_in = din("ck", [DEPTH, 2, 128, 256])
    cv_in = din("cv", [DEPTH, 2, 128, 256])
    sp_in = din("sp", [DEPTH, 2, 15, 1024])
    c_ident = din("c_ident", [128, 128])
    c_R = din("c_R", [128, 128])
    c_cos = din("c_cos", [128, TK])
    c_sin = din("c_sin", [128, TK])
    c_kbias = din("c_kbias", [128, 24])
    c_invcnt = din("c_invcnt", [128, 8, 16])
    norm_mix = din("norm_mix", [DEPTH, D])
    w_in = din("w_in", [DEPTH, D, IN_W])
    q_norm = din("q_norm", [DEPTH, 64])
    k_norm = din("k_norm", [DEPTH, 64])
    sinks = din("sinks", [DEPTH, 16])
    pool_w = din("pool_w", [DEPTH, 4, 256, 256])
    pool_scale = din("pool_scale", [DEPTH, 1024])
    w_attn_br = din("w_attn_br", [DEPTH, 1024, D])
    w_pool_br = din("w_pool_br", [DEPTH, 1024, D])
    w_out = din("w_out", [DEPTH, D, D])
    norm_ffn = din("norm_ffn", [DEPTH, D])
    ffn_w_gate_up = din("ffn_w_gate_up", [1, D, 2 * DFF])
    ffn_w_down = din("ffn_w_down", [1, DFF, D])
    moe_router = din("moe_router", [1, D, NEXP])
    if tiny_moe:
        moe_w_gate_up = din("moe_w_gate_up", [1, NEXP, 8, 8])
        moe_w_down = din("moe_w_down", [1, NEXP, 8, 8])
    else:
        moe_w_gate_up = din("moe_w_gate_up", [1, NEXP, D, 2 * DFF])
        moe_w_down = din("moe_w_down", [1, NEXP, DFF, D])
    norm_ple = din("norm_ple", [DEPTH, D])
    w_ple = din("w_ple", [DEPTH, 256, D])
    w_ple_gate = din("w_ple_gate", [DEPTH, D, D])
    y_out = dout("y", [1152, D])
    nkp = dout("nkp", [DEPTH, 128, 256])
    nvp = dout("nvp", [DEPTH, 128, 256])
    npp = dout("npp", [DEPTH, 15, 1024])
    nks = dout("nks", [DEPTH, 2, 128, 256])
    nvs = dout("nvs", [DEPTH, 2, 128, 256])
    nps = dout("nps", [DEPTH, 2, 15, 1024])
    xs = nc.dram_tensor("xspill", [128, NCH * XW], F32, kind="Internal").ap()

    es.enter_context(nc.allow_low_precision("bf16 matmul operands with fp32 accumulation"))
    es.enter_context(nc.allow_non_contiguous_dma("small per-partition vector loads"))
    P = Prog(nc, es, debug)
    PE, ACT, DVE, POOL, SP = P.PE, P.ACT, P.DVE, P.POOL, P.SP

    def sb(name, shape, dt):
        return es.enter_context(nc.sbuf_tensor(name, list(shape), dt))

    arena = sb("arena", [128, NCH * XW], F32)
    Hb = sb("Hb", [128, NCH * TK], BF16)
    ring = sb("ring", [128, 4, 4096], BF16)
    bigst = sb("bigst", [128, 2, 1024], F32)
    tmp = sb("tmp", [128, 8, 512], F32)
    actbuf = sb("actbuf", [128, 2, 2, XW], BF16)
    sqb = sb("sqb", [128, 2, 512], BF16)
    ident = sb("ident", [128, 128], F32)
    Rm = sb("Rm", [128, 128], F32)
    Rg = sb("Rg", [128, 2, 128], F32)
    BD = sb("BD", [128, 128], F32)
    ones_f = sb("ones_f", [128, 128], F32)
    ones_b = sb("ones_b", [128, 128], BF16)
    onesz = sb("onesz", [128, 192], BF16)
    gam = sb("gam", [128, 3, 16], F32)
    pscale = sb("pscale", [128, 8], F32)
    qkg = sb("qkg", [128, 2], F32)
    esink = sb("esink", [128, 8], F32)
    sk16 = sb("sk16", [128, 16], F32)
    kbias = sb("kbias", [128, 24], F32)
    invcnt = sb("invcnt", [128, 8, 16], F32)
    histT = sb("histT", [128, 8, 2, 16], F32)
    kTc = sb("kTc", [128, 4, 2, 128], BF16)
    pTa = sb("pTa", [128, 4, 128], BF16)
    pTb = sb("pTb", [128, 4, 128], BF16)
    dent = sb("dent", [128, 4, 64], F32)
    qbd = sb("qbd", [128, 4, 128], BF16)
    pst = sb("pst", [16, 1, 3, 128], F32)
    rtr = sb("rtr", [128, 16, 8], F32)
    lg = sb("lg", [128, 9, 8], F32)
    comb = sb("comb", [128, 9, 8], F32)
    rsm = sb("rsm", [128, 9, 8], F32)
    psb = [es.enter_context(nc.psum_tensor("ps%d" % i, [128, 512], F32)) for i in range(8)]

    xv = arena[:, :].rearrange("p (k c) -> p k c", k=NCH)

    def xap(k, c0, c1):
        if c1 <= 128:
            return tmp[:, 0:4, :].rearrange("p a b -> p (a b)").rearrange("p (k c) -> p k c", k=NCH)[:, k, c0:c1]
        assert c0 >= 128
        return xv[:, k, c0 - 128:c1 - 128]

    def xck(k, c0, c1):
        if c1 <= 128:
            return [("tmp", s) for s in range(4)]
        return cks("x", k, c0, c1)

    hv = Hb[:, :].rearrange("p (k c) -> p k c", k=NCH)
    xlo = Hb[:, 0:2 * 8 * XW].bitcast(F32).rearrange("p (k c) -> p k c", k=8)
    A_bf = arena[:, 0:10240].bitcast(BF16)
    kT = A_bf[:, 0:4 * TK].rearrange("p (g c) -> p g c", g=4)
    Vze = A_bf[:, 5632:5632 + 11 * 768].rearrange("p (b g d) -> p b g d", b=11, g=4)
    cosT = arena[:, 7040:7040 + TK]
    sinT = arena[:, 8448:8448 + TK]
    m_t = A_bf[:, 0:NCH * XW].rearrange("p (k c) -> p k c", k=NCH)
    ub3 = arena[:, 0:3 * 1536].rearrange("p (a c) -> p a c", a=3)
    qo = arena[:, 10240:15360].bitcast(BF16).rearrange("p (k c) -> p k c", k=8)
    dp = arena[:, 15360:20480].bitcast(BF16).rearrange("p (k c) -> p k c", k=8)
    Vzo = arena[:, 15360:15360 + 11 * 384].bitcast(BF16).rearrange("p (b g d) -> p b g d", b=11, g=4)

    Vzc = arena[:, 15360 + 4224:15360 + 4224 + 768].bitcast(BF16).rearrange("p (s g d) -> p s g d", s=2, g=4)

    kf = arena[:, 10240:10240 + 1024].rearrange("p (g c) -> p g c", g=4)

    def qoap(j, c0, c1):
        return qo[:, j, c0 - 128:c1 - 128]

    def dpap(j, c0, c1):
        return dp[:, j, c0 - 128:c1 - 128]

    def map_(k, c0, c1):
        return m_t[:, k, c0 - 128:c1 - 128]

    s_init = P.dsem("d_init")
    s_lay = P.dsem("d_lay")
    s_big = [P.dsem("d_big0"), P.dsem("d_big1")]
    s_ring = [P.dsem("d_ring%d" % i) for i in range(4)]
    for d in s_ring:
        d.ring = True
    s_spill = P.dsem("d_spill")
    s_xr = [P.dsem("d_xr%d" % i) for i in range(8)]
    s_out = P.dsem("d_out")
    s_pst = [P.dsem("d_pst0"), P.dsem("d_pst1")]
    s_tst = [P.dsem("d_tst%d" % i) for i in range(8)]

    def bank():
        b = P.bank_i
        P.bank_i = (b + 1) % 8
        return b

    def tslot():
        s = P.tmp_i
        P.tmp_i = (s + 1) % 8
        return s

    ring_i = [0]

    def wload(parts):
        s = ring_i[0]
        ring_i[0] = (s + 1) % 4
        key = ("ring", s)
        slot = ring[:, s, :]
        for dst_fn, src in parts:
            P.dma(POOL, dst_fn(slot), src, s_ring[s], (), [key])
        return slot, key

    def w_k16(Wl, col0):
        slot, key = wload([(lambda s: s.rearrange("p (a b) -> p a b", a=16),
                            Wl[:, col0:col0 + 256].rearrange("(kc p) n -> p kc n", p=128))])
        return slot.rearrange("p (a b) -> p a b", a=16), key

    dbg_list = []

    def dbg(name, ap, shape, dt, reads):
        if name not in debug:
            return
        o = dout("dbg_" + name, shape, dt)
        P.dma(SP, o, ap, s_out, reads, (), commit_reads=False)
        P.barrier()
        dbg_list.append(name)

    P.dma(SP, ident[:, :], c_ident, s_init, (), [("ident",)])
    P.dma(SP, Rm[:, :], c_R, s_init, (), [("Rm",)])
    P.dma(SP, kbias[:, :], c_kbias, s_init, (), [("kbias",)])
    P.dma(SP, invcnt[:, :, :], c_invcnt, s_init, (), [("invcnt",)])
    P.group_commit(s_init, [("ident",), ("Rm",), ("kbias",), ("invcnt",)])
    P.memset(DVE, BD[:, :], 0.0, [("BD",)])
    P.memset(DVE, BD[0:64, 0:64], 1.0, [("BD",)])
    P.memset(DVE, BD[64:128, 64:128], 1.0, [("BD",)])
    P.memset(DVE, ones_f[:, :], 1.0, [("ones_f",)])
    P.memset(DVE, ones_b[:, :], 1.0, [("ones_b",)])
    P.memset(DVE, onesz[:, :], 0.0, [("onesz",)])
    P.memset(DVE, onesz[:, 64:128], 1.0, [("onesz",)])
    P.memset(DVE, histT[:, :, :, :], 0.0, [("histT", 0), ("histT", 1)])
    P.memset(DVE, qbd[:, :, :], 0.0, [("qbd", r_) for r_ in range(4)])
    P.memset(DVE, pTb[:, :, :], 0.0, [("pTb", r_) for r_ in range(4)])

    def rmsnorm(l, which, gvec, rng_blocks, want_f32=None):
        for (c0, c1) in rng_blocks:
            w = c1 - c0
            b = bank()
            P.job([("ps", b)])
            for k in range(NCH):
                s = k % 2
                P.act(sqb[:, s, 0:w], xap(k, c0, c1), AF.Square, xck(k, c0, c1), [("sqb", s)])
                P.mm(psb[b][:, 0:w], ones_b[:, :], sqb[:, s, 0:w], k == 0, k == NCH - 1,
                     [("ones_b",), ("sqb", s)], last=(k == NCH - 1), inc=True)
            ts_ = tslot()
            while ts_ < 4 and c1 <= 128:
                ts_ = tslot()
            if c1 <= 128:
                ts_ = 4
            rs = tmp[:, ts_, 0:w]
            P.act(rs, psb[b][:, 0:w], AF.Sqrt, [("ps", b)], [("tmp", ts_)], bias=EPS, scale=1.0 / D)
            P.op(DVE, lambda e, rs=rs: e.reciprocal(out=rs, in_=rs), [("tmp", ts_)], [("tmp", ts_)])
            for k in range(NCH):
                P.stt(hv[:, k, c0:c1], xap(k, c0, c1), gam[:, which, k:k + 1], rs, ALU.mult, ALU.mult,
                      xck(k, c0, c1) + [("tmp", ts_), ("gam", l, which)], cks("h", k, c0, c1))
            if want_f32 is not None:
                want_f32(c0, c1, rs, ("tmp", ts_))

    def load_vec16(dst, src_row, key):
        P.dma(SP, dst, src_row.rearrange("(kc p) -> p kc", p=128), s_lay, (), [key])


    def qk_chain(b, w, c0, c1, which, out_bf, out_bf_keys, out_f=None, out_f_keys=()):
        t_qs, t_sq, t_rs, t_t1 = tslot(), tslot(), tslot(), tslot()
        qs, sq, rs, t1 = (tmp[:, t, 0:w] for t in (t_qs, t_sq, t_rs, t_t1))
        P.copy(ACT, qs, psb[b][:, 0:w], [("ps", b)], [("tmp", t_qs)])
        P.act(sq, psb[b][:, 0:w], AF.Square, [("ps", b)], [("tmp", t_sq)])
        b2 = bank()
        P.job([("ps", b2)])
        P.mm(psb[b2][:, 0:w], BD[:, :], sq, True, True, [("BD",), ("tmp", t_sq)], last=True)
        b3 = bank()
        P.job([("ps", b3)])
        P.mm(psb[b3][:, 0:w], Rg[:, which, :], qs, True, True, [("Rg",), ("tmp", t_qs)], last=True)
        P.act(rs, psb[b2][:, 0:w], AF.Sqrt, [("ps", b2)], [("tmp", t_rs)], bias=EPS, scale=1.0 / 64)
        P.op(DVE, lambda e: e.reciprocal(out=rs, in_=rs), [("tmp", t_rs)], [("tmp", t_rs)])
        P.stt(t1, qs, qkg[:, which:which + 1], cosT[:, c0:c1], ALU.mult, ALU.mult,
              [("tmp", t_qs), ("qkg",), ("cos",)], [("tmp", t_t1)])
        P.tt(sq, psb[b3][:, 0:w], sinT[:, c0:c1], ALU.mult, [("ps", b3), ("sin",)], [("tmp", t_sq)])
        P.tt(t1, t1, sq, ALU.add, [("tmp", t_t1), ("tmp", t_sq)], [("tmp", t_t1)])
        P.tt(out_bf, t1, rs, ALU.mult, [("tmp", t_t1), ("tmp", t_rs)], out_bf_keys)
        if out_f is not None:
            o0 = max(c0, 1152)
            P.tt(out_f(o0, c1), t1[:, o0 - c0:w], rs[:, o0 - c0:w], ALU.mult, [("tmp", t_t1), ("tmp", t_rs)], out_f_keys)

    def layer(l):
        F0, F1 = FR[l]
        K0, K1 = KVR[l]
        fblocks = blocks(F0, F1)
        if l == 0:
            kvblocks = [(0, 128)] + blocks(128, TK)
        else:
            kvblocks = blocks(K0, K1)
        Wl = w_in[l]

        load_vec16(gam[:, 0, :], norm_mix[l], ("gam", l, 0))
        load_vec16(gam[:, 1, :], norm_ffn[l], ("gam", l, 1))
        load_vec16(gam[:, 2, :], norm_ple[l], ("gam", l, 2))
        P.dma(SP, pscale[:, :], pool_scale[l].rearrange("(kc p) -> p kc", p=128), s_lay, (), [("pscale",)])
        for hh in range(2):
            P.dma(SP, qkg[hh * 64:(hh + 1) * 64, 0:1], q_norm[l].rearrange("(p o) -> p o", o=1), s_lay, (), [("qkg",)])
            P.dma(SP, qkg[hh * 64:(hh + 1) * 64, 1:2], k_norm[l].rearrange("(p o) -> p o", o=1), s_lay, (), [("qkg",)])
        P.dma(SP, sk16[:, :], sinks[l:l + 1, :].to_broadcast([128, 16]), s_lay, (), [("sk16",)])
        P.group_commit(s_lay, [("gam", l, 0), ("gam", l, 1), ("gam", l, 2), ("pscale",), ("qkg",), ("sk16",)])
        for hh in range(2):
            P.act(esink[hh * 64:(hh + 1) * 64, :], sk16[hh * 64:(hh + 1) * 64, :].rearrange("p (j two) -> p j two", two=2)[:, :, hh],
                  AF.Exp, [("sk16",)], [("esink",)])
        for which in range(2):
            P.ts(Rg[:, which, :], Rm[:, :], qkg[:, which:which + 1], ALU.mult, [("Rm",), ("qkg",)], [("Rg",)])

        if l == 0:
            for rb in range(11):
                for half in range(2):
                    s = half
                    P.dma(SP, bigst[:, s, :], xin[rb * 128:(rb + 1) * 128, half * 1024:(half + 1) * 1024], s_big[s],
                          (), [("bigst", s)])
                    for jj in range(2):
                        b = bank()
                        P.job([("ps", b)])
                        for kk in range(4):
                            P.mm(psb[b][:, kk * 128:(kk + 1) * 128], bigst[:, s, (jj * 4 + kk) * 128:(jj * 4 + kk + 1) * 128],
                                 ident[:, :], True, True, [("bigst", s), ("ident",)], last=(kk == 3))
                        k0 = half * 8 + jj * 4
                        src = psb[b][:, :].rearrange("p (a c) -> p a c", a=4)
                        if rb == 0:
                            dst = tmp[:, 0:4, :].rearrange("p a b -> p (a b)").rearrange("p (k c) -> p k c", k=NCH)[:, k0:k0 + 4, :]
                            wk = [("tmp", t) for t in range(4)]
                        else:
                            dst = xv[:, k0:k0 + 4, (rb - 1) * 128:rb * 128]
                            wk = [("x", k0 + i, rb) for i in range(4)]
                        P.copy(P.alt(), dst, src, [("ps", b)], wk)
            P.tmp_i = 4

        rmsnorm(l, 0, None, kvblocks)
        dbg("h%d" % l, Hb[:, :], [128, NCH * TK], BF16, [("h", k, cb) for k in range(NCH) for cb in range(11)])
        for k in range(NCH):
            P.dma(SP, xs[:, k * XW:(k + 1) * XW], xv[:, k, :], s_spill, cks("x", k, 128, TK), [("xs", k)])
        P.barrier()
        if stop_after == "h":
            return False

        P.dma(SP, cosT, c_cos, s_lay, (), [("cos",)])
        P.dma(SP, sinT, c_sin, s_lay, (), [("sin",)])
        P.group_commit(s_lay, [("cos",), ("sin",)])
        P.memset(DVE, Vze[:, :, :, :], 0.0, [("Vze", cb) for cb in range(11)])
        P.memset(DVE, Vzo[:, :, :, :], 0.0, [("Vzo", cb) for cb in range(11)])
        P.memset(DVE, Vzc[:, :, :, :], 0.0, [("Vzc",)])
        for s in range(2):
            st = bigst[:, s, 0:512].rearrange("p (g t d) -> p g t d", g=4, t=2)
            for t in range(2):
                P.dma(SP, st[:, :, t, :], ck_in[l, s].rearrange("k (g d) -> k g d", g=4), s_big[s], (), [("bigst", s)])
            b = bank()
            P.job([("ps", b)])
            for g in range(4):
                P.mm(psb[b][:, g * 128:(g + 1) * 128], bigst[:, s, g * 128:(g + 1) * 128], ident[:, :], True, True,
                     [("bigst", s), ("ident",)], last=(g == 3))
            P.copy(DVE, kTc[:, :, s, :], psb[b][:, :].rearrange("p (g k) -> p g k", g=4), [("ps", b)], [("kTc", s)])
            P.dma(SP, bigst[:, s, 0:256], cv_in[l, s], s_big[s], (), [("bigst", s)])
            P.copy(DVE, Vzc[:, s, :, 64:128], bigst[:, s, 0:256].rearrange("p (g d) -> p g d", g=4),
                   [("bigst", s)], [("Vzc",)])
            P.dma(SP, nks[l, s, 0:64, :], ck_in[l, s, 64:128, :], s_out, (), ())
            P.dma(SP, nvs[l, s, 0:64, :], cv_in[l, s, 64:128, :], s_out, (), ())

        if stop_after == "kvpre":
            return False
        for t in range(2):
            def dstf(gg_, dupi):
                return lambda s_: s_.rearrange("p (kc gg u d) -> p kc gg u d", kc=16, gg=2, u=2)[:, :, gg_, dupi, :]

            def srcf(gg_):
                c_ = C_K + 128 * t + 64 * gg_
                return Wl[:, c_:c_ + 64].rearrange("(kc p) d -> p kc d", p=128)
            slot, wkey = wload([(dstf(gg_, du), srcf(gg_)) for gg_ in range(2) for du in range(2)])
            wt = slot.rearrange("p (kc n) -> p kc n", kc=16)
            for gg in range(2):
                g = 2 * t + gg
                for (c0, c1) in blocks(K0, K1):
                    w = c1 - c0
                    b = bank()
                    P.job([("ps", b)])
                    for k in range(NCH):
                        P.mm(psb[b][:, 0:w], wt[:, k, gg * 128:(gg + 1) * 128], hv[:, k, c0:c1], k == 0, k == NCH - 1,
                             [wkey] + cks("h", k, c0, c1), last=(k == NCH - 1))
                    outf = (lambda o0, o1, g=g: kf[:, g, o0 - 1152:o1 - 1152]) if c1 > 1152 else None
                    qk_chain(b, w, c0, c1, 1, kT[:, g, c0:c1], cks("kT", g, c0, c1), outf, [("kf", g)])
        if stop_after == "k":
            return False
        wv, wvkey = w_k16(Wl, C_V)
        for grid in range(2):
            for cb in range(11):
                t0 = cb * 128 + 64 * grid
                t1 = min(t0 + 128, TK)
                if t0 < K0 or t0 >= TK:
                    continue
                nt = t1 - t0
                b = bank()
                P.job([("ps", b)])
                for k in range(NCH):
                    P.mm(psb[b][0:nt, 0:256], hv[:, k, t0:t1], wv[:, k, :], k == 0, k == NCH - 1,
                         [wvkey] + cks("h", k, t0, t1), last=(k == NCH - 1))
                Vz = Vze if grid == 0 else Vzo
                P.copy(DVE, Vz[0:nt, cb, :, 64:128], psb[b][0:nt, 0:256].rearrange("p (g d) -> p g d", g=4),
                       [("ps", b)], [("Vze" if grid == 0 else "Vzo", cb)])
                if grid == 0 and cb in (9, 10):
                    ts_ = tslot()
                    P.copy(DVE, tmp[:, ts_, 0:256], psb[b][:, 0:256], [("ps", b)], [("tmp", ts_)])
                    if cb == 9:
                        P.dma(SP, nvp[l], tmp[:, ts_, 0:256], s_tst[ts_], [("tmp", ts_)], ())
                    else:
                        for s in range(2):
                            P.dma(SP, nvs[l, s, 64:128, :], tmp[64 * s:64 * s + 64, ts_, 0:256], s_tst[ts_], [("tmp", ts_)], ())
        if stop_after == "v":
            return False
        for part in range(2):
            b = bank()
            P.job([("ps", b)])
            for g in range(4):
                P.mm(psb[b][:, g * 64:(g + 1) * 64], kf[0:64, g, part * 128:(part + 1) * 128], ident[0:64, 0:64], True, True,
                     [("kf", g), ("ident",)], last=(g == 3))
            ts_ = tslot()
            P.copy(DVE, tmp[:, ts_, 0:256], psb[b][:, 0:256], [("ps", b)], [("tmp", ts_)])
            if part == 0:
                P.dma(SP, nkp[l], tmp[:, ts_, 0:256], s_tst[ts_], [("tmp", ts_)], ())
            else:
                for s in range(2):
                    P.dma(SP, nks[l, s, 64:128, :], tmp[64 * s:64 * s + 64, ts_, 0:256], s_tst[ts_], [("tmp", ts_)], ())

        for t in range(4):
            wt, wkey = w_k16(Wl, C_Q + 256 * t)
            for jj in range(2):
                j = 2 * t + jj
                for (c0, c1) in fblocks:
                    w = c1 - c0
                    b = bank()
                    P.job([("ps", b)])
                    for k in range(NCH):
                        P.mm(psb[b][:, 0:w], wt[:, k, jj * 128:(jj + 1) * 128], hv[:, k, c0:c1], k == 0, k == NCH - 1,
                             [wkey] + cks("h", k, c0, c1), last=(k == NCH - 1))
                    qk_chain(b, w, c0, c1, 0, qoap(j, c0, c1), cks("qo", j, c0, c1))
        dbg("q%d" % l, arena[:, 10240:15360], [128, 5120], F32, [("qo", j, cb) for j in range(8) for cb in range(11)])
        dbg("kT%d" % l, arena[:, 0:2816], [128, 2816], F32, [("kT", g, cb) for g in range(4) for cb in range(11)])
        if stop_after == "q":
            return False

        units = []
        for c in range(F0 // 64, 20):
            if c % 2 == 0:
                VA, VB = (Vze, (c - 2) // 2, "Vze"), (Vze, c // 2, "Vze")
            else:
                VA, VB = (Vzo, (c - 3) // 2, "Vzo"), (Vzo, (c - 1) // 2, "Vzo")
            units.append(dict(q0=64 * c, kA=lambda g, c=c: kT[:, g, 64 * (c - 2):64 * c], kAk=lambda g, c=c: cks("kT", g, 64 * (c - 2), 64 * c),
                              VA=VA, bA=c - 2, kB0=64 * c, VB=VB, bB=c))
        for s in range(2):
            VB = (Vze, 10, "Vze") if s == 0 else (Vzo, 10, "Vzo")
            units.append(dict(q0=C_S[s], kA=lambda g, s=s: kTc[:, g, s, :], kAk=lambda g, s=s: [("kTc", s)],
                              VA=(None, s, "Vzc"), bA=23, kB0=C_S[s], VB=VB, bB=23))
        items = [(u, j) for u in units for j in range(8)]

        def stage_a(i):
            u, j = items[i]
            q0 = u["q0"]
            g = j // 2
            r = i % 4
            qkeys = cks("qo", j, q0, q0 + 64)
            b = bank()
            P.job([("ps", b)])
            kA = u["kA"](g)
            kB = kT[:, g, u["kB0"]:u["kB0"] + 64]
            kBk = cks("kT", g, u["kB0"], u["kB0"] + 64)
            qa = qoap(j, q0, q0 + 64)
            P.copy(ACT, qbd[0:64, r, 0:64], qa[0:64, :], qkeys, [("qbd", r)])
            P.copy(ACT, qbd[64:128, r, 64:128], qa[64:128, :], qkeys, [("qbd", r)])
            P.mm(psb[b][:, 0:128], kA, qbd[:, r, :], True, True, u["kAk"](g) + [("qbd", r)])
            P.mm(psb[b][0:64, 128:256], kB, qbd[:, r, :], True, True, kBk, last=True)
            P.act(pTa[:, r, :], psb[b][:, 0:128], AF.Exp, [("ps", b), ("kbias",)], [("pTa", r)],
                  bias=kbias[:, u["bA"]:u["bA"] + 1], scale=0.125)
            P.act(pTb[0:64, r, :], psb[b][0:64, 128:256], AF.Exp, [("ps", b), ("kbias",)], [("pTb", r)],
                  bias=kbias[0:64, u["bB"]:u["bB"] + 1], scale=0.125)

        def stage_b(i):
            u, j = items[i]
            q0 = u["q0"]
            g = j // 2
            r = i % 4
            qkeys = cks("qo", j, q0, q0 + 64)
            qa = qoap(j, q0, q0 + 64)
            VzA, ia, na = u["VA"]
            VzB, ib, nb = u["VB"]
            if VzA is None:
                vA = Vzc[:, ia, g, :]
                vAk = [("Vzc",)]
            else:
                vA = VzA[:, ia, g, :]
                vAk = [(na, ia)]
            vB = VzB[:, ib, g, :]
            vBk = [(nb, ib)]
            b2 = bank()
            P.job([("ps", b2)])
            P.mm(psb[b2][:, 0:64], vA[:, 64:192], pTa[:, r, 0:64], True, False, vAk + [("pTa", r)])
            P.mm(psb[b2][:, 0:64], vA[:, 0:128], pTa[:, r, 64:128], False, False)
            P.mm(psb[b2][:, 0:64], vB[:, 64:192], pTb[:, r, 0:64], False, False, vBk + [("pTb", r)])
            P.mm(psb[b2][:, 0:64], vB[:, 0:128], pTb[:, r, 64:128], False, True)
            P.mm(psb[b2][:, 64:128], onesz[:, 64:192], pTa[:, r, 0:64], True, False, [("onesz",)])
            P.mm(psb[b2][:, 64:128], onesz[:, 0:128], pTa[:, r, 64:128], False, False)
            P.mm(psb[b2][:, 64:128], onesz[:, 64:192], pTb[:, r, 0:64], False, False)
            P.mm(psb[b2][:, 64:128], onesz[:, 0:128], pTb[:, r, 64:128], False, True, last=True)
            dn = dent[:, r, :]
            P.ts(dn, psb[b2][:, 64:128], esink[:, j:j + 1], ALU.add, [("ps", b2), ("esink",)], [("dent", r)])
            P.op(DVE, lambda e, dn=dn: e.reciprocal(out=dn, in_=dn), [("dent", r)], [("dent", r)])
            P.tt(qa, psb[b2][:, 0:64], dn, ALU.mult, [("ps", b2), ("dent", r)], qkeys)

        stage_a(0)
        for i in range(1, len(items)):
            stage_a(i)
            stage_b(i - 1)
        stage_b(len(items) - 1)
        dbg("o%d" % l, arena[:, 10240:15360], [128, 5120], F32, [("qo", j, cb) for j in range(8) for cb in range(11)])
        P.barrier()
        if stop_after == "attn":
            return False

        for s in range(2):
            P.dma(SP, bigst[0:15, s, :], sp_in[l, s], s_big[s], (), [("bigst", s)])
            b = bank()
            P.job([("ps", b)])
            for uc in range(8):
                P.mm(psb[b][:, uc * 16 + 1:uc * 16 + 16], bigst[0:15, s, uc * 128:(uc + 1) * 128], ident[0:15, 0:15], True, True,
                     [("bigst", s), ("ident",)], last=(uc == 7))
            P.copy(DVE, histT[:, :, s, 1:16], psb[b][:, 0:128].rearrange("p (u c) -> p u c", u=8)[:, :, 1:16],
                   [("ps", b)], [("histT", s)])
        ub, sA, sB = ub3[:, 0, :], ub3[:, 1, :], ub3[:, 2, :]
        UW = 1440

        def umap(c0, c1):
            out = []
            segs = ((0, 1280, 0), (1280, 1344, 1296), (1344, 1408, 1376))
            for (a0, a1, o) in segs:
                lo, hi = max(c0, a0), min(c1, a1)
                if lo < hi:
                    out.append((lo - c0, hi - lo, o + lo - a0))
            return out

        for t in range(4):
            wt, wkey = w_k16(Wl, C_U + 256 * t)
            for jj in range(2):
                uc = 2 * t + jj
                gi = uc // 2
                for (c0, c1) in blocks(K0, K1):
                    w = c1 - c0
                    b = bank()
                    P.job([("ps", b)])
                    for k in range(NCH):
                        P.mm(psb[b][:, 0:w], wt[:, k, jj * 128:(jj + 1) * 128], hv[:, k, c0:c1], k == 0, k == NCH - 1,
                             [wkey] + cks("h", k, c0, c1), last=(k == NCH - 1))
                    for (so, n, uo) in umap(c0, c1):
                        P.copy(ACT, ub[:, uo:uo + n], psb[b][:, so:so + n], [("ps", b)], [("ub",)])
                for s in range(2):
                    P.copy(DVE, ub[:, 1280 + 80 * s:1296 + 80 * s], histT[:, uc, s, :], [("histT", s)], [("ub",)])
                if l == 1:
                    P.memset(DVE, ub[:, 0:128], 0.0, [("ub",)])
                P.memset(DVE, ub[:, 1280:1281], 0.0, [("ub",)])
                P.memset(DVE, ub[:, 1360:1361], 0.0, [("ub",)])
                P.tt(sA[:, 1:UW], ub[:, 1:UW], ub[:, 0:UW - 1], ALU.add, [("ub",)], [("sA",)])
                fin, fk = sA, ("sA",)
                if gi >= 1:
                    P.tt(sB[:, 3:UW], sA[:, 3:UW], sA[:, 1:UW - 2], ALU.add, [("sA",)], [("sB",)])
                    fin, fk = sB, ("sB",)
                if gi >= 2:
                    P.tt(sA[:, 7:UW], sB[:, 7:UW], sB[:, 3:UW - 4], ALU.add, [("sB",)], [("sA",)])
                    fin, fk = sA, ("sA",)
                if gi >= 3:
                    P.tt(sB[:, 15:UW], sA[:, 15:UW], sA[:, 7:UW - 8], ALU.add, [("sA",)], [("sB",)])
                    fin, fk = sB, ("sB",)
                wnd = 2 ** (gi + 1)
                for (so, n, uo) in umap(F0, F1):
                    cc0 = F0 + so
                    P.stt(dpap(uc, cc0, cc0 + n), fin[:, uo:uo + n], 1.0 / wnd, ub[:, uo:uo + n], ALU.mult, ALU.subtract,
                          [fk, ("ub",)], cks("dp", uc, cc0, cc0 + n))
                ts_ = tslot()
                tf = tmp[:, ts_, 0:16]
                P.tt(tf, fin[:, C_MAIN:C_MAIN + 16], invcnt[:, uc, :], ALU.mult, [fk, ("invcnt",)], [("tmp", ts_)])
                P.tt(dpap(uc, C_MAIN, C_MAIN + 16), tf, ub[:, C_MAIN:C_MAIN + 16], ALU.subtract, [("tmp", ts_), ("ub",)],
                     cks("dp", uc, C_MAIN, C_MAIN + 16))
                b = bank()
                P.job([("ps", b)])
                offs = (1265, 1296 + 49, 1376 + 49)
                for i, o in enumerate(offs):
                    P.mm(psb[b][0:15, i * 128:(i + 1) * 128], ub[:, o:o + 15], ident[:, :], True, True, [("ub",), ("ident",)],
                         last=(i == 2))
                ps_ = 0
                P.copy(DVE, pst[0:15, ps_, :, :], psb[b][0:15, 0:384].rearrange("p (a c) -> p a c", a=3), [("ps", b)], [("pst", ps_)])
                P.dma(SP, npp[l][:, uc * 128:(uc + 1) * 128], pst[0:15, ps_, 0, :], s_pst[ps_], [("pst", ps_)], ())
                for s in range(2):
                    P.dma(SP, nps[l, s][:, uc * 128:(uc + 1) * 128], pst[0:15, ps_, 1 + s, :], s_pst[ps_], [("pst", ps_)], ())
        dbg("d%d" % l, arena[:, 15360:20480], [128, 5120], F32, [("dp", j, cb) for j in range(8) for cb in range(11)])

        slot, wkey = wload([(lambda s_: s_[:, 0:2048].rearrange("p (a b) -> p a b", a=8),
                             pool_w[l].rearrange("g (cc p) d -> p (g cc) d", p=128))])
        wt = slot[:, 0:2048].rearrange("p (a b) -> p a b", a=8)
        for gi in range(4):
            for (c0, c1) in fblocks:
                w = c1 - c0
                bs = []
                for dh in range(2):
                    b = bank()
                    bs.append(b)
                    P.job([("ps", b)])
                    for cc in range(2):
                        P.mm(psb[b][:, 0:w], wt[:, 2 * gi + cc, dh * 128:(dh + 1) * 128], dpap(2 * gi + cc, c0, c1), cc == 0, cc == 1,
                             [wkey] + cks("dp", 2 * gi + cc, c0, c1), last=(cc == 1))
                for dh in range(2):
                    P.ts(dpap(2 * gi + dh, c0, c1), psb[bs[dh]][:, 0:w], pscale[:, 2 * gi + dh:2 * gi + dh + 1], ALU.mult,
                         [("ps", bs[dh]), ("pscale",)], cks("dp", 2 * gi + dh, c0, c1))
        dbg("pl%d" % l, arena[:, 15360:20480], [128, 5120], F32, [("dp", j, cb) for j in range(8) for cb in range(11)])
        P.barrier()
        if stop_after == "pool":
            return False

        for pr in range(8):
            def half(i):
                return lambda s_: s_[:, 2048 * i:2048 * (i + 1)].rearrange("p (a b) -> p a b", a=8)
            slot, wbk = wload([(half(0), w_attn_br[l][:, pr * 256:(pr + 1) * 256].rearrange("(kc p) n -> p kc n", p=128)),
                               (half(1), w_pool_br[l][:, pr * 256:(pr + 1) * 256].rearrange("(kc p) n -> p kc n", p=128))])
            wab = slot[:, 0:2048].rearrange("p (a b) -> p a b", a=8)
            wpb = slot[:, 2048:4096].rearrange("p (a b) -> p a b", a=8)
            wga, wgak = w_k16(Wl, C_GA + 256 * pr)
            wgb, wgbk = w_k16(Wl, C_GB + 256 * pr)
            for jj in range(2):
                n = 2 * pr + jj
                for (c0, c1) in fblocks:
                    w = c1 - c0
                    ba, bga, bc, bgb = bank(), bank(), bank(), bank()
                    P.job([("ps", ba)])
                    for k in range(8):
                        P.mm(psb[ba][:, 0:w], wab[:, k, jj * 128:(jj + 1) * 128], qoap(k, c0, c1), k == 0, k == 7,
                             [wbk] + cks("qo", k, c0, c1), last=(k == 7))
                    P.job([("ps", bga)])
                    for k in range(NCH):
                        P.mm(psb[bga][:, 0:w], wga[:, k, jj * 128:(jj + 1) * 128], hv[:, k, c0:c1], k == 0, k == NCH - 1,
                             [wgak] + cks("h", k, c0, c1), last=(k == NCH - 1))
                    P.job([("ps", bc)])
                    for k in range(8):
                        P.mm(psb[bc][:, 0:w], wpb[:, k, jj * 128:(jj + 1) * 128], dpap(k, c0, c1), k == 0, k == 7,
                             [wbk] + cks("dp", k, c0, c1), last=(k == 7))
                    P.job([("ps", bgb)])
                    for k in range(NCH):
                        P.mm(psb[bgb][:, 0:w], wgb[:, k, jj * 128:(jj + 1) * 128], hv[:, k, c0:c1], k == 0, k == NCH - 1,
                             [wgbk] + cks("h", k, c0, c1), last=(k == NCH - 1))
                    t1_, t2_ = tslot(), tslot()
                    s1, s2 = tmp[:, t1_, 0:w], tmp[:, t2_, 0:w]
                    P.act(s1, psb[bga][:, 0:w], AF.Sigmoid, [("ps", bga)], [("tmp", t1_)])
                    P.act(s2, psb[bgb][:, 0:w], AF.Sigmoid, [("ps", bgb)], [("tmp", t2_)])
                    P.tt(s1, s1, psb[ba][:, 0:w], ALU.mult, [("tmp", t1_), ("ps", ba)], [("tmp", t1_)])
                    P.tt(s2, s2, psb[bc][:, 0:w], ALU.mult, [("tmp", t2_), ("ps", bc)], [("tmp", t2_)])
                    P.tt(map_(n, c0, c1), s1, s2, ALU.add, [("tmp", t1_), ("tmp", t2_)], cks("m", n, c0, c1))
        P.barrier()

        xr_i = 0
        for pr in range(8):
            wt, wkey = w_k16(w_out[l], 256 * pr)
            for jj in range(2):
                n = 2 * pr + jj
                for (c0, c1) in fblocks:
                    w = c1 - c0
                    b = bank()
                    P.job([("ps", b)])
                    for k in range(NCH):
                        P.mm(psb[b][:, 0:w], wt[:, k, jj * 128:(jj + 1) * 128], map_(k, c0, c1), k == 0, k == NCH - 1,
                             [wkey] + cks("m", k, c0, c1), last=(k == NCH - 1))
                    ts_ = xr_i % 8
                    xr_i += 1
                    P.dma(SP, tmp[:, ts_, 0:w], xs[:, n * XW + c0 - 128:n * XW + c1 - 128], s_xr[ts_], [("xs", n)], [("tmp", ts_)])
                    if n >= 8:
                        dst, dk = xv[:, n, c0 - 128:c1 - 128], cks("x", n, c0, c1)
                    else:
                        dst, dk = xlo[:, n, c0 - 128:c1 - 128], cks("xlo", n, c0, c1)
                    P.tt(dst, tmp[:, ts_, 0:w], psb[b][:, 0:w], ALU.add, [("tmp", ts_), ("ps", b)], dk)
        P.barrier()
        for n in range(8):
            for (c0, c1) in fblocks:
                P.copy(P.alt(), xv[:, n, c0 - 128:c1 - 128], xlo[:, n, c0 - 128:c1 - 128], cks("xlo", n, c0, c1), cks("x", n, c0, c1))
        P.barrier()
        dbg("x1_%d" % l, arena[:, :], [128, NCH * XW], F32, [("x", k, cb) for k in range(NCH) for cb in range(1, 11)])
        if stop_after == "mix":
            return False

        P.tmp_i = 0
        if l % 2 == 0:
            rmsnorm(l, 1, None, fblocks)
        else:
            P.dma(SP, rtr[:, :, :], moe_router[0].rearrange("(kc p) e -> p kc e", p=128), s_lay, (), [("rtr",)])
            state = {"n": 0}
            ntb = (F1 - F0) // 128

            def router_block(c0, c1, rs, rsk):
                for i in range((c1 - c0) // 128):
                    tb_ = (c0 - F0) // 128 + i
                    a0 = c0 + i * 128
                    lb = bank()
                    P.job([("ps", lb)])
                    for k in range(NCH):
                        ts2 = 6 + (state["n"] % 2)
                        state["n"] += 1
                        hf = tmp[:, ts2, 0:128]
                        P.stt(hf, xap(k, a0, a0 + 128), gam[:, 1, k:k + 1], rs[:, i * 128:(i + 1) * 128], ALU.mult, ALU.mult,
                              xck(k, a0, a0 + 128) + [rsk, ("gam", l, 1)], [("tmp", ts2)])
                        P.mm(psb[lb][:, 0:8], hf, rtr[:, k, :], k == 0, k == NCH - 1, [("tmp", ts2), ("rtr",)],
                             last=(k == NCH - 1), inc=True)
                    P.copy(DVE, lg[:, tb_, :], psb[lb][:, 0:8], [("ps", lb)], [("lg", tb_)])

            P.tmp_i = 0
            rmsnorm(l, 1, None, fblocks, want_f32=router_block)
            P.tmp_i = 0
            for tb_ in range(ntb):
                L = lg[:, tb_, :]
                sc = rsm[:, tb_, :]
                P.op(DVE, lambda e, L=L, sc=sc: e.tensor_reduce(out=sc[:, 0:1], in_=L, axis=AX.X, op=ALU.max), [("lg", tb_)], [("rsm", tb_)])
                P.ts(sc[:, 2:3], sc[:, 0:1], -1.0, ALU.mult, [("rsm", tb_)], [("rsm", tb_)])
                c_ = comb[:, tb_, :]
                P.ts(c_, L, sc[:, 0:1], ALU.is_equal, [("lg", tb_), ("rsm", tb_)], [("comb", tb_)], s2=-1e30, op1=ALU.mult)
                P.tt(c_, c_, L, ALU.add, [("comb", tb_), ("lg", tb_)], [("comb", tb_)])
                P.op(DVE, lambda e, c_=c_, sc=sc: e.tensor_reduce(out=sc[:, 1:2], in_=c_, axis=AX.X, op=ALU.max), [("comb", tb_)], [("rsm", tb_)])
                P.ts(c_, L, sc[:, 1:2], ALU.is_ge, [("lg", tb_), ("rsm", tb_)], [("comb", tb_)])
                ex = rsm[:, tb_, 3:4]
                P.act(lg[:, tb_, :], L, AF.Exp, [("lg", tb_), ("rsm", tb_)], [("lg", tb_)], bias=sc[:, 2:3], scale=1.0)
                P.tt(c_, c_, lg[:, tb_, :], ALU.mult, [("comb", tb_), ("lg", tb_)], [("comb", tb_)])
                P.op(DVE, lambda e, c_=c_, ex=ex: e.tensor_reduce(out=ex, in_=c_, axis=AX.X, op=ALU.add), [("comb", tb_)], [("rsm", tb_)])
                P.op(DVE, lambda e, ex=ex: e.reciprocal(out=ex, in_=ex), [("rsm", tb_)], [("rsm", tb_)])
                P.ts(c_, c_, ex, ALU.mult, [("comb", tb_), ("rsm", tb_)], [("comb", tb_)])
        dbg("h2_%d" % l, Hb[:, :], [128, NCH * TK], BF16, [("h", k, cb) for k in range(NCH) for cb in range(11)])
        if l % 2 == 1:
            dbg("comb", comb[:, :, :], [128, 9, 8], F32, [("comb", t) for t in range(9)])
        if stop_after == "h2":
            return False

        cmbt = tmp[:, 5:8, :].rearrange("p a b -> p (a b)")

        def cmbgen(e):
            ntb = (F1 - F0) // 128
            for q4 in range(0, ntb, 4):
                nb_ = min(4, ntb - q4)
                b = bank()
                P.job([("ps", b)])
                for i in range(nb_):
                    tb_ = q4 + i
                    dg = tmp[:, i % 2, 0:128]
                    P.ts(dg, ident[:, :], comb[:, tb_, e:e + 1], ALU.mult, [("ident",), ("comb", tb_)], [("tmp", i % 2)])
                    P.mm(psb[b][:, i * 128:(i + 1) * 128], ones_f[:, :], dg, True, True, [("ones_f",), ("tmp", i % 2)],
                         last=(i == nb_ - 1), inc=True)
                P.copy(ACT, cmbt[:, q4 * 128:(q4 + nb_) * 128], psb[b][:, 0:nb_ * 128], [("ps", b)], [("cmb",)])

        def ffn_all(specs):
            seq = [(Wgu, Wd, e, grp) for (Wgu, Wd, e) in specs for grp in range(NHID // 2)]

            def gu(i):
                Wgu, Wd, e, grp = seq[i]
                par = i % 2
                j0 = 2 * grp
                if grp == 0 and e is not None:
                    cmbgen(e)
                wg, wgk = w_k16(Wgu, j0 * 128)
                wu, wuk = w_k16(Wgu, DFF + j0 * 128)
                for jj in range(2):
                    for (c0, c1) in fblocks:
                        w = c1 - c0
                        bg, bu = bank(), bank()
                        P.job([("ps", bg)])
                        for k in range(NCH):
                            P.mm(psb[bg][:, 0:w], wg[:, k, jj * 128:(jj + 1) * 128], hv[:, k, c0:c1], k == 0, k == NCH - 1,
                                 [wgk] + cks("h", k, c0, c1), last=(k == NCH - 1))
                        P.job([("ps", bu)])
                        for k in range(NCH):
                            P.mm(psb[bu][:, 0:w], wu[:, k, jj * 128:(jj + 1) * 128], hv[:, k, c0:c1], k == 0, k == NCH - 1,
                                 [wuk] + cks("h", k, c0, c1), last=(k == NCH - 1))
                        ts_ = 2 + (P.tmp_i % 3)
                        P.tmp_i += 1
                        sg = tmp[:, ts_, 0:w]
                        P.act(sg, psb[bg][:, 0:w], AF.Silu, [("ps", bg)], [("tmp", ts_)])
                        if e is not None:
                            P.tt(sg, sg, cmbt[:, c0 - F0:c1 - F0], ALU.mult, [("tmp", ts_), ("cmb",)], [("tmp", ts_)])
                        P.tt(actbuf[:, par, jj, c0 - 128:c1 - 128], sg, psb[bu][:, 0:w], ALU.mult, [("tmp", ts_), ("ps", bu)],
                             cks("act%d" % par, jj, c0, c1))

            def down(i):
                Wgu, Wd, e, grp = seq[i]
                par = i % 2
                j0 = 2 * grp
                slot, wdk = wload([(lambda s_: s_.rearrange("p (a b) -> p a b", a=2),
                                    Wd[j0 * 128:(j0 + 2) * 128, :].rearrange("(jj p) n -> p jj n", p=128))])
                wd = slot.rearrange("p (a b) -> p a b", a=2)
                for n in range(NCH):
                    for (c0, c1) in fblocks:
                        w = c1 - c0
                        b = bank()
                        P.job([("ps", b)])
                        for jj in range(2):
                            P.mm(psb[b][:, 0:w], wd[:, jj, n * 128:(n + 1) * 128], actbuf[:, par, jj, c0 - 128:c1 - 128], jj == 0, jj == 1,
                                 [wdk] + cks("act%d" % par, jj, c0, c1), last=(jj == 1))
                        P.tt(xv[:, n, c0 - 128:c1 - 128], xv[:, n, c0 - 128:c1 - 128], psb[b][:, 0:w], ALU.add,
                             cks("x", n, c0, c1) + [("ps", b)], cks("x", n, c0, c1))

            gu(0)
            for i in range(1, len(seq)):
                gu(i)
                down(i - 1)
            down(len(seq) - 1)

        if l % 2 == 0:
            ffn_all([(ffn_w_gate_up[l // 2], ffn_w_down[l // 2], None)])
        else:
            ffn_all([(moe_w_gate_up[l // 2, e], moe_w_down[l // 2, e], e) for e in range(NEXP)])
        dbg("x2_%d" % l, arena[:, :], [128, NCH * XW], F32, [("x", k, cb) for k in range(NCH) for cb in range(1, 11)])
        P.barrier()
        if stop_after == "ffn":
            return False

        P.tmp_i = 0
        rmsnorm(l, 2, None, fblocks)
        pT = actbuf[:, :, :, :].rearrange("p a b c -> p (a b c)")[:, 0:2 * XW].rearrange("p (k c) -> p k c", k=2)
        for rb in range((F0 - 128) // 128, 10):
            s = rb % 2
            P.dma(SP, bigst[:, s, 0:256], pin[l, rb * 128:(rb + 1) * 128, :], s_big[s], (), [("bigst", s)])
            b = bank()
            P.job([("ps", b)])
            for kc in range(2):
                P.mm(psb[b][:, kc * 128:(kc + 1) * 128], bigst[:, s, kc * 128:(kc + 1) * 128], ident[:, :], True, True,
                     [("bigst", s), ("ident",)], last=(kc == 1))
            P.copy(DVE, pT[:, :, rb * 128:(rb + 1) * 128], psb[b][:, 0:256].rearrange("p (k c) -> p k c", k=2),
                   [("ps", b)], [("pT", rb + 1)])
        for pr in range(8):
            slot, wpk = wload([(lambda s_: s_.rearrange("p (a b) -> p a b", a=2),
                                w_ple[l].rearrange("(kc p) n -> p kc n", p=128))])
            wp = slot.rearrange("p (a b) -> p a b", a=2)
            wt, wkey = w_k16(w_ple_gate[l], 256 * pr)
            for jj in range(2):
                n = 2 * pr + jj
                for (c0, c1) in fblocks:
                    w = c1 - c0
                    bg, bp = bank(), bank()
                    P.job([("ps", bg)])
                    for k in range(NCH):
                        P.mm(psb[bg][:, 0:w], wt[:, k, jj * 128:(jj + 1) * 128], hv[:, k, c0:c1], k == 0, k == NCH - 1,
                             [wkey] + cks("h", k, c0, c1), last=(k == NCH - 1))
                    P.job([("ps", bp)])
                    for kc in range(2):
                        P.mm(psb[bp][:, 0:w], wp[:, kc, n * 128:(n + 1) * 128], pT[:, kc, c0 - 128:c1 - 128], kc == 0, kc == 1,
                             [wpk] + [("pT", cb) for cb in range(c0 // 128, c1 // 128)], last=(kc == 1))
                    ts_ = tslot()
                    sg = tmp[:, ts_, 0:w]
                    P.act(sg, psb[bg][:, 0:w], AF.Sigmoid, [("ps", bg)], [("tmp", ts_)])
                    P.tt(sg, sg, psb[bp][:, 0:w], ALU.mult, [("tmp", ts_), ("ps", bp)], [("tmp", ts_)])
                    P.tt(xv[:, n, c0 - 128:c1 - 128], xv[:, n, c0 - 128:c1 - 128], sg, ALU.add,
                         cks("x", n, c0, c1) + [("tmp", ts_)], cks("x", n, c0, c1))
        dbg("x3_%d" % l, arena[:, :], [128, NCH * XW], F32, [("x", k, cb) for k in range(NCH) for cb in range(1, 11)])
        P.barrier()
        return True

    ok = True
    for l in range(nlayers):
        ok = layer(l)
        if not ok:
            break

    if ok:
        for rb in range(9):
            for half in range(2):
                s = half
                for jj in range(2):
                    b = bank()
                    P.job([("ps", b)])
                    for kk in range(4):
                        k = half * 8 + jj * 4 + kk
                        P.mm(psb[b][:, kk * 128:(kk + 1) * 128], xv[:, k, 128 + rb * 128:256 + rb * 128], ident[:, :], True, True,
                             [("x", k, rb + 2), ("ident",)], last=(kk == 3))
                    P.copy(P.alt(), bigst[:, s, jj * 512:(jj + 1) * 512], psb[b][:, :], [("ps", b)], [("bigst", s)])
                P.dma(SP, y_out[rb * 128:(rb + 1) * 128, half * 1024:(half + 1) * 1024], bigst[:, s, :], s_big[s], [("bigst", s)], ())

    for d in P.dsems:
        if d.issued > 0:
            P.need(SP, Tok(d.sem, d.issued))
    for E in (PE, ACT, DVE):
        if E.count > 0:
            P.need(SP, Tok(E.sem, E.count))

    def run_q(E):
        def f(e):
            for it in E.q:
                if it[0] == "w":
                    e.wait_ge(it[1], it[2])
                else:
                    ins = it[1](e)
                    if it[2] is not None:
                        ins.then_inc(it[2], it[3])
        return f

    with nc.Block() as block:
        block.tensor(run_q(PE))
        block.scalar(run_q(ACT))
        block.vector(run_q(DVE))
        block.gpsimd(run_q(POOL))
        block.sync(run_q(SP))
    es.close()
    stats = {E.name: (len(E.q), E.count) for E in P.engs}
    return nc, dbg_list, stats


def _consts(core):
    qd = core % 4
    s0 = qd * 1024
    pos = np.zeros(TK, np.float64)
    pos[:1280] = s0 - 256 + np.arange(1280)
    pos[1280:1344] = PAST + np.arange(64)
    pos[1344:1408] = PAST + np.arange(64)
    inv = (ROPE_THETA ** (-np.arange(0, 16, 2, dtype=np.float32) / 16)).astype(np.float32)
    ang = pos.astype(np.float32)[None, :] * inv[:, None]
    cos = np.ones((128, TK), np.float32)
    sin = np.zeros((128, TK), np.float32)
    for hh in range(2):
        for r in range(2):
            cos[hh * 64 + r * 8:hh * 64 + r * 8 + 8] = np.cos(ang)
            sin[hh * 64 + r * 8:hh * 64 + r * 8 + 8] = np.sin(ang)
    R = np.zeros((128, 128), np.float32)
    for hh in range(2):
        for i in range(8):
            R[hh * 64 + i + 8, hh * 64 + i] = -1.0
            R[hh * 64 + i, hh * 64 + i + 8] = 1.0
    kb = np.zeros((128, 24), np.float32)
    for cs in range(20):
        p = s0 - 256 + 64 * cs + np.arange(128)
        kb[:, cs] = np.where(p < 0, NEG, 0.0)
    ic = np.zeros((128, 8, 16), np.float32)
    for uc in range(8):
        w = 2 ** (uc // 2 + 1)
        p = s0 + np.arange(16)
        ic[:, uc, :] = (1.0 / np.minimum(w, p + 1))[None, :]
    return dict(c_ident=np.eye(128, dtype=np.float32), c_R=R, c_cos=cos, c_sin=sin, c_kbias=kb, c_invcnt=ic)


def _core_inputs(core, inp):
    b, qd = core // 4, core % 4
    s0 = qd * 1024
    m = {}
    xin = np.zeros((TK, D), np.float32)
    lo = s0 - 256
    a = max(lo, 0)
    xin[a - lo:1280] = inp["x_prompt"][b, a:s0 + 1024]
    xin[1280:1344] = inp["x_sample"][2 * core]
    xin[1344:1408] = inp["x_sample"][2 * core + 1]
    m["xin"] = xin
    pin = np.zeros((DEPTH, XW, 256), np.float32)
    lo = s0 - 128
    a = max(lo, 0)
    pin[:, a - lo:1152] = inp["p_prompt"][:, b, a:s0 + 1024]
    pin[:, 1152:1216] = inp["p_sample"][:, 2 * core]
    pin[:, 1216:1280] = inp["p_sample"][:, 2 * core + 1]
    m["pin"] = pin
    m["ck"] = np.ascontiguousarray(inp["cache_k"][:, 2 * core:2 * core + 2].reshape(DEPTH, 2, 128, 256))
    m["cv"] = np.ascontiguousarray(inp["cache_v"][:, 2 * core:2 * core + 2].reshape(DEPTH, 2, 128, 256))
    m["sp"] = np.ascontiguousarray(inp["state_pool"][:, 2 * core:2 * core + 2])
    m.update(_consts(core))
    return m


WEIGHTS = ["norm_mix", "w_in", "q_norm", "k_norm", "sinks", "pool_w", "pool_scale", "w_attn_br", "w_pool_br", "w_out",
           "norm_ffn", "ffn_w_gate_up", "ffn_w_down", "moe_router", "moe_w_gate_up", "moe_w_down", "norm_ple", "w_ple",
           "w_ple_gate"]


def kernel(**inputs):
    inp = {k: np.asarray(v) for k, v in inputs.items()}
    nc, _, _ = build_program()
    wts = {k: np.ascontiguousarray(inp[k], dtype=np.float32) for k in WEIGHTS}
    in_maps = []
    for c in range(NCORES):
        m = _core_inputs(c, inp)
        m.update(wts)
        in_maps.append(m)
    res = run_bass_kernel_spmd(nc, in_maps, core_ids=list(range(NCORES)))
    r = res.results
    y_prompt = np.zeros((2, 4096, D), np.float32)
    y_sample = np.zeros((16, 64, D), np.float32)
    nkp = np.zeros((DEPTH, 2, 128, 4, 64), np.float32)
    nvp = np.zeros((DEPTH, 2, 128, 4, 64), np.float32)
    npp = np.zeros((DEPTH, 2, 15, 1024), np.float32)
    nks = np.zeros((DEPTH, 16, 128, 4, 64), np.float32)
    nvs = np.zeros((DEPTH, 16, 128, 4, 64), np.float32)
    nps = np.zeros((DEPTH, 16, 15, 1024), np.float32)
    for c in range(NCORES):
        b, qd = c // 4, c % 4
        y_prompt[b, qd * 1024:(qd + 1) * 1024] = r[c]["y"][0:1024]
        y_sample[2 * c] = r[c]["y"][1024:1088]
        y_sample[2 * c + 1] = r[c]["y"][1088:1152]
        if qd == 3:
            nkp[:, b] = r[c]["nkp"].reshape(DEPTH, 128, 4, 64)
            nvp[:, b] = r[c]["nvp"].reshape(DEPTH, 128, 4, 64)
            npp[:, b] = r[c]["npp"]
        nks[:, 2 * c:2 * c + 2] = r[c]["nks"].reshape(DEPTH, 2, 128, 4, 64)
        nvs[:, 2 * c:2 * c + 2] = r[c]["nvs"].reshape(DEPTH, 2, 128, 4, 64)
        nps[:, 2 * c:2 * c + 2] = r[c]["nps"]
    return (y_prompt, y_sample, nkp, nvp, npp, nks, nvs, nps)
```
